# Optimizing a Trainium2 kernel written in Bass

```python
import jax, jax.numpy as jnp
from jax import lax
import numpy as np

D_MODEL = 1024
BATCH = 4
SEQ = 4096
DEPTH = 1

N_SB_HEADS = 8
SB_HEAD_DIM = 64
SB_WIDTH = N_SB_HEADS * SB_HEAD_DIM
POOL_WINDOWS = (2, 4, 8, 16)
N_POOL_GROUPS = len(POOL_WINDOWS)
POOL_GROUP_DIM = 128
POOL_WIDTH = N_POOL_GROUPS * POOL_GROUP_DIM
MIX_WIDTH = SB_WIDTH + POOL_WIDTH
IN_PROJ_WIDTH = 3 * SB_WIDTH + POOL_WIDTH
Q_BLOCK = 128
N_EXPERT_GROUPS = 4
EXPERTS_PER_GROUP = 4
N_EXPERTS = N_EXPERT_GROUPS * EXPERTS_PER_GROUP
TOP_K_IN_GROUP = 2
EXPERT_HIDDEN = 512
DEEPNORM_ALPHA = (2.0 * DEPTH) ** 0.25
DEEPNORM_BETA = (8.0 * DEPTH) ** -0.25
LN_EPS = 1e-5
N_MOD = 6

kernel_name = "hybrid_stickbreak_pool_hmoe_deepnorm_adaln"


def layer_norm(x, gain=None, bias=None):
    xf = x.astype(jnp.float32)
    mu = jnp.mean(xf, axis=-1, keepdims=True)
    var = jnp.mean(jnp.square(xf - mu), axis=-1, keepdims=True)
    y = (xf - mu) * lax.rsqrt(var + LN_EPS)
    if gain is not None:
        y = y * gain.astype(jnp.float32) + bias.astype(jnp.float32)
    return y.astype(x.dtype)


def stick_breaking_attention(q, k, v):
    seq = q.shape[2]
    scale = SB_HEAD_DIM ** -0.5
    outs = []
    for blk in range(seq // Q_BLOCK):
        start = blk * Q_BLOCK
        end = start + Q_BLOCK
        qb = q[:, :, start:end]
        kb = k[:, :, :end]
        vb = v[:, :, :end]
        z = jnp.einsum('bhqd,bhkd->bhqk', qb, kb).astype(jnp.float32) * scale
        q_pos = start + jnp.arange(Q_BLOCK)[:, None]
        k_pos = jnp.arange(end)[None, :]
        causal = k_pos < q_pos
        log_beta = jnp.where(causal, jax.nn.log_sigmoid(z), -jnp.inf)
        log_not_beta = jnp.where(causal, jax.nn.log_sigmoid(-z), 0.0)
        after = lax.cumsum(log_not_beta, axis=3, reverse=True) - log_not_beta
        att = jnp.exp(log_beta + after)
        outs.append(jnp.einsum('bhqk,bhkd->bhqd', att.astype(vb.dtype), vb))
    return jnp.concatenate(outs, axis=2)


def causal_pool_minus_self(xg, window):
    seq = xg.shape[1]
    xf = xg.astype(jnp.float32)
    cs = jnp.cumsum(xf, axis=1)
    lag = jnp.pad(cs, ((0, 0), (window, 0), (0, 0)))[:, :seq]
    count = jnp.minimum(jnp.arange(seq) + 1, window).astype(jnp.float32)
    mean = (cs - lag) / count[None, :, None]
    return (mean - xf).astype(xg.dtype)


def multiscale_pool(p, w_pool, pool_scale):
    b, s, _ = p.shape
    groups = [causal_pool_minus_self(p[..., g * POOL_GROUP_DIM:(g + 1) * POOL_GROUP_DIM], w)
              for g, w in enumerate(POOL_WINDOWS)]
    pooled = jnp.stack(groups, axis=2)
    mixed = jnp.einsum('bsgc,gce->bsge', pooled, w_pool).reshape(b, s, POOL_WIDTH)
    return mixed * pool_scale


def hierarchical_moe(u, w_rg, b_rg, w_re, b_re, w_gate, w_up, w_down):
    t, d = u.shape
    g_logits = (u @ w_rg).astype(jnp.float32) + b_rg.astype(jnp.float32)
    g_prob = jax.nn.softmax(g_logits, axis=-1)
    g_p, g_idx = lax.top_k(g_prob, 1)
    e_logits = ((u @ w_re).astype(jnp.float32) + b_re.astype(jnp.float32)).reshape(
        t, N_EXPERT_GROUPS, EXPERTS_PER_GROUP)
    e_sel = jnp.take_along_axis(e_logits, g_idx[:, :, None], axis=1)[:, 0]
    e_top, e_idx = lax.top_k(e_sel, TOP_K_IN_GROUP)
    e_w = jax.nn.softmax(e_top, axis=-1) * g_p
    expert_id = g_idx * EXPERTS_PER_GROUP + e_idx
    combine = jnp.sum(jax.nn.one_hot(expert_id, N_EXPERTS, dtype=jnp.float32) * e_w[..., None],
                      axis=1)
    y = jnp.zeros((t, d), jnp.float32)
    for e in range(N_EXPERTS):
        h = jax.nn.silu(u @ w_gate[e]) * (u @ w_up[e])
        y = y + combine[:, e:e + 1] * (h @ w_down[e]).astype(jnp.float32)
    return y.astype(u.dtype)


def setup_inputs(seed: int = 0) -> dict:
    key = jax.random.key(seed)
    ks = jax.random.split(key, 20)
    f32 = jnp.float32
    n = lambda k, shape, s: jax.random.normal(k, shape, f32) * s
    d = D_MODEL
    w_in = n(ks[4], (DEPTH, d, IN_PROJ_WIDTH), d ** -0.5)
    v_cols = jnp.zeros((IN_PROJ_WIDTH,), f32).at[2 * SB_WIDTH:3 * SB_WIDTH].set(1.0)
    w_in = w_in * (1.0 + (DEEPNORM_BETA - 1.0) * v_cols)
    return {
        "x": n(ks[0], (BATCH, SEQ, d), 1.0),
        "c": n(ks[1], (BATCH, d), 1.0),
        "w_ada": n(ks[2], (DEPTH, d, N_MOD * d), 0.1 * d ** -0.5),
        "b_ada": n(ks[3], (DEPTH, N_MOD * d), 0.01),
        "w_in": w_in,
        "w_pool": n(ks[5], (DEPTH, N_POOL_GROUPS, POOL_GROUP_DIM, POOL_GROUP_DIM), POOL_GROUP_DIM ** -0.5),
        "pool_scale": 1.0 + n(ks[6], (DEPTH, POOL_WIDTH), 0.05),
        "w_out": n(ks[7], (DEPTH, MIX_WIDTH, d), DEEPNORM_BETA * MIX_WIDTH ** -0.5),
        "ln1_g": 1.0 + n(ks[8], (DEPTH, d), 0.02),
        "ln1_b": n(ks[9], (DEPTH, d), 0.02),
        "w_router_group": n(ks[10], (DEPTH, d, N_EXPERT_GROUPS), d ** -0.5),
        "b_router_group": n(ks[11], (DEPTH, N_EXPERT_GROUPS), 0.01),
        "w_router_expert": n(ks[12], (DEPTH, d, N_EXPERTS), d ** -0.5),
        "b_router_expert": n(ks[13], (DEPTH, N_EXPERTS), 0.01),
        "w_gate": n(ks[14], (DEPTH, N_EXPERTS, d, EXPERT_HIDDEN), d ** -0.5),
        "w_up": n(ks[15], (DEPTH, N_EXPERTS, d, EXPERT_HIDDEN), DEEPNORM_BETA * d ** -0.5),
        "w_down": n(ks[16], (DEPTH, N_EXPERTS, EXPERT_HIDDEN, d), DEEPNORM_BETA * EXPERT_HIDDEN ** -0.5),
        "ln2_g": 1.0 + n(ks[17], (DEPTH, d), 0.02),
        "ln2_b": n(ks[18], (DEPTH, d), 0.02),
    }


def reference(x, c, w_ada, b_ada, w_in, w_pool, pool_scale, w_out, ln1_g, ln1_b,
              w_router_group, b_router_group, w_router_expert, b_router_expert,
              w_gate, w_up, w_down, ln2_g, ln2_b):
    b, s, d = x.shape
    heads = lambda t: t.reshape(b, s, N_SB_HEADS, SB_HEAD_DIM).transpose(0, 2, 1, 3)
    for layer in range(DEPTH):
        mod = jax.nn.silu(c) @ w_ada[layer] + b_ada[layer]
        shift1, scale1, gate1, shift2, scale2, gate2 = [m[:, None, :] for m in jnp.split(mod, N_MOD, axis=-1)]

        u = layer_norm(x) * (1.0 + scale1) + shift1
        proj = u @ w_in[layer]
        q, k, v, p = jnp.split(proj, [SB_WIDTH, 2 * SB_WIDTH, 3 * SB_WIDTH], axis=-1)
        o_sb = stick_breaking_attention(heads(q), heads(k), heads(v))
        o_sb = o_sb.transpose(0, 2, 1, 3).reshape(b, s, SB_WIDTH)
        o_pool = multiscale_pool(p, w_pool[layer], pool_scale[layer])
        mixed = jnp.concatenate([o_sb, o_pool], axis=-1) @ w_out[layer]
        x = layer_norm(DEEPNORM_ALPHA * x + (1.0 + gate1) * mixed, ln1_g[layer], ln1_b[layer])

        u = layer_norm(x) * (1.0 + scale2) + shift2
        y = hierarchical_moe(u.reshape(b * s, d), w_router_group[layer], b_router_group[layer],
                             w_router_expert[layer], b_router_expert[layer],
                             w_gate[layer], w_up[layer], w_down[layer]).reshape(b, s, d)
        x = layer_norm(DEEPNORM_ALPHA * x + (1.0 + gate2) * y, ln2_g[layer], ln2_b[layer])
    return x
```

```python
import os
import numpy as np
from contextlib import ExitStack
import concourse.bass as bass
import concourse.mybir as mybir
from concourse.bass_utils import run_bass_kernel_spmd

F32 = mybir.dt.float32
BF16 = mybir.dt.bfloat16
AF = mybir.ActivationFunctionType
ALU = mybir.AluOpType
AX = mybir.AxisListType

D = 1024
ALPHA = 2.0 ** 0.25
EPS = 1e-5
NEG = -30000.0
BIG = 1.0e9
NEXP = 16


class Res:
    __slots__ = ("name", "lw", "rd", "sem", "ndma")

    def __init__(self, name):
        self.name = name
        self.lw = None
        self.rd = {}
        self.sem = None
        self.ndma = 0


def _tok_key(t):
    return (t[0], t[1] if t[0] == "c" else id(t[1]))


class Prog:
    ENGS = ("pe", "act", "dve", "pool", "sp")

    def __init__(self):
        self.ops = {e: [] for e in self.ENGS}
        self.waited = {e: {} for e in self.ENGS}
        self.flag = {e: set() for e in self.ENGS}
        self.dma_res = []

    @staticmethod
    def _merge(dst, tok):
        k = _tok_key(tok)
        old = dst.get(k)
        if old is None or old[2] < tok[2]:
            dst[k] = tok

    def alias(self, new, olds):
        for o in olds:
            for t in o.rd.values():
                self._merge(new.rd, t)
            if o.lw is not None:
                self._merge(new.rd, o.lw)

    def op(self, eng, fn, rd=(), wr=(), dma=None):
        deps = {}
        for r in rd:
            if r.lw is not None:
                self._merge(deps, r.lw)
        for w in wr:
            if w.lw is not None:
                self._merge(deps, w.lw)
            for t in w.rd.values():
                self._merge(deps, t)
        waits = []
        wd = self.waited[eng]
        for k, t in deps.items():
            if t[0] == "c" and t[1] == eng and eng == "pe":
                continue
            if wd.get(k, -1) >= t[2]:
                continue
            wd[k] = t[2]
            waits.append(t)
            if t[0] == "c":
                self.flag[t[1]].add(t[2])
        idx = len(self.ops[eng])
        if dma is not None:
            if dma.ndma == 0:
                self.dma_res.append(dma)
            dma.ndma += 1
            tok = ("d", dma, dma.ndma)
        else:
            tok = ("c", eng, idx)
        self.ops[eng].append((fn, waits, dma))
        for r in rd:
            self._merge(r.rd, tok)
        for w in wr:
            w.lw = tok
            w.rd = {}
        return tok

    def emit(self, nc):
        with ExitStack() as st:
            sems = {e: st.enter_context(nc.semaphore("s_" + e)) for e in self.ENGS}
            for i, r in enumerate(self.dma_res):
                r.sem = st.enter_context(nc.semaphore("d%d" % i))
            val = {}
            for e in self.ENGS:
                cnt = 0
                m = {}
                for i in range(len(self.ops[e])):
                    if i in self.flag[e]:
                        cnt += 1
                        m[i] = cnt
                val[e] = m
            block = st.enter_context(nc.Block())

            def run(e, h):
                fl = self.flag[e]
                for i, (fn, waits, dma) in enumerate(self.ops[e]):
                    for t in waits:
                        if t[0] == "c":
                            h.wait_ge(sems[t[1]], val[t[1]][t[2]])
                        else:
                            h.wait_ge(t[1].sem, 16 * t[2])
                    if fn is None:
                        continue
                    ins = fn(h)
                    if dma is not None:
                        ins.then_inc(dma.sem, 16)
                    elif i in fl:
                        ins.then_inc(sems[e], 1)

            @block.tensor
            def _(h):
                run("pe", h)

            @block.scalar
            def _(h):
                run("act", h)

            @block.vector
            def _(h):
                run("dve", h)

            @block.gpsimd
            def _(h):
                run("pool", h)

            @block.sync
            def _(h):
                run("sp", h)
                for r in self.dma_res:
                    h.wait_ge(r.sem, 16 * r.ndma)


def build(NSLOT=8, phases=5):
    NV = 512 * NSLOT
    NO = 256 * NSLOT
    NT = 4 * NSLOT
    NOT = 2 * NSLOT
    NTG = NO // 512
    nc = bass.Bass("TRN2", target_bir_lowering=False)

    def din(name, shape):
        return nc.dram_tensor(name, shape, F32, kind="ExternalInput").ap()

    xv = din("xv", [NV, D])
    cT_d = din("cT", [128, 8])
    wada_d = din("w_ada", [D, 6 * D])
    badaT_d = din("badaT", [128, 48])
    brow_d = din("brow", [6, 128, D])
    win_d = din("w_in", [D, 2048])
    wpool_d = din("w_pool", [4, 128, 128])
    pscT_d = din("pscT", [128, 4])
    wout_d = din("w_out", [D, D])
    wr_d = din("wr", [D, 20])
    brb_d = din("brb", [128, 20])
    wg_d = din("w_gate", [NEXP, D, 512])
    wu_d = din("w_up", [NEXP, D, 512])
    wd_d = din("w_down", [NEXP, 512, D])
    identf_d = din("identf", [128, 128])
    tri_d = din("tri", [128, 128])
    ones_d = din("ones", [128, 128])
    onesn_d = din("onesn", [128, 128])
    mask_d = din("maskb", [2, 128, 512])
    kbias_d = din("kbias", [128, NT])
    kpad_d = din("kpad", [128, 512])
    valid_d = din("valid", [128, 1])
    invc_d = din("invc", [128, 4, 256])
    out_d = nc.dram_tensor("out", [NO, D], F32, kind="ExternalOutput").ap()

    p = Prog()
    st = ExitStack()
    ABYTES = 211968
    arena = st.enter_context(nc.sbuf_tensor("arena", [128, ABYTES // 4], F32))
    ps = st.enter_context(nc.psum_tensor("ps", [128, 4096], F32))
    a_f32 = arena
    a_bf = arena.bitcast(BF16)
    ps_bf = ps.bitcast(BF16)

    def carve(off, dt, shape):
        es = 4 if dt == F32 else 2
        assert off % 4 == 0
        n = 1
        for s in shape[1:]:
            n *= s
        base = a_f32 if dt == F32 else a_bf
        v = base[:, off // es: off // es + n]
        if len(shape) == 3:
            v = v.rearrange("p (a b) -> p a b", b=shape[2])
        return v, off + n * es

    KB = 1024
    KT, _ = carve(0, BF16, [128, 4, NV])
    Vt, _ = carve(32 * KB, BF16, [128, NT, 512])
    acc, _ = carve(0, F32, [128, NOT, D])
    R2 = 64 * KB
    QT, _ = carve(R2, BF16, [128, 4, NO])
    OP, _ = carve(R2 + 16 * KB, BF16, [128, 4, NO])
    OS, _ = carve(R2 + 32 * KB, BF16, [128, 4, NO])
    wgb, wub, wdb = [], [], []
    for b in range(2):
        o = R2 + 24 * KB * b
        t, o = carve(o, BF16, [128, 8, 512]); wgb.append(t)
        t, o = carve(o, BF16, [128, 8, 512]); wub.append(t)
        t, o = carve(o, BF16, [128, 4, D]); wdb.append(t)
    R3 = 112 * KB
    win, _ = carve(R3, BF16, [128, 8, 2048])
    u2T, _ = carve(R3, BF16, [128, 8, NO])
    R4 = 144 * KB
    wab = [carve(R2 + 32 * KB + 8 * KB * b, BF16, [128, 8, 512])[0] for b in range(2)]
    uT, _ = carve(R4, BF16, [128, 8, 512])
    xn, _ = carve(R4 + 8 * KB, BF16, [128, 4, D])
    wout, _ = carve(R4, BF16, [128, 8, D])
    o = 160 * KB
    identb, o = carve(o, BF16, [128, 128])
    trib, o = carve(o, BF16, [128, 128])
    onesb, o = carve(o, BF16, [128, 128])
    onesn, o = carve(o, BF16, [128, 128])
    maskb, o = carve(o, BF16, [128, 2, 512])
    kbias, o = carve(o, F32, [128, 32])
    valid, o = carve(o, F32, [128, 4])
    wpool, o = carve(o, BF16, [128, 4, 128])
    wr, o = carve(o, F32, [128, 8, 20])
    brb, o = carve(o, F32, [128, 20])
    wrh, o = carve(o, BF16, [128, 8, 20])
    wrl, o = carve(o, BF16, [128, 8, 20])
    pscT, o = carve(o, F32, [128, 4])
    badaT, o = carve(o, F32, [128, 48])
    cT, o = carve(o, F32, [128, 8])
    scT, o = carve(o, BF16, [128, 8])
    modT, o = carve(o, F32, [128, 4, 8])
    g1b, o = carve(o, F32, [128, D])
    g2b, o = carve(o, F32, [128, D])
    combAll, o = carve(o, F32, [128, NOT, 16])
    NSC = 8
    st_t, mv_t, rs_t, nm_t = [], [], [], []
    for k in range(NSC):
        t, o = carve(o, F32, [128, 12]); st_t.append(t)
        t, o = carve(o, F32, [128, 2]); mv_t.append(t)
        t, o = carve(o, F32, [128, 2]); rs_t.append(t)
        t, o = carve(o, F32, [128, 2]); nm_t.append(t)
    kpadb, o = carve(o, BF16, [128, 512])
    epsc, o = carve(o, F32, [128, 4])
    W0 = o
    WEND = ABYTES
    assert W0 + 28 * KB <= WEND, W0
    bcA, _ = carve(W0 + 20 * KB, F32, [128, D])
    bcB, _ = carve(W0 + 24 * KB, F32, [128, D])

    def R(n):
        return Res(n)

    r_ps = [R("ps%d" % i) for i in range(8)]

    def bank(i, lo=0, hi=512):
        return ps[:, 512 * i + lo: 512 * i + hi]

    ln_ctr = [0]

    def ln_stats(src, src_res, eps, eps_col=0):
        k = ln_ctr[0] % NSC
        ln_ctr[0] += 1
        stt, mv, rs, nm = st_t[k], mv_t[k], rs_t[k], nm_t[k]
        rr = lnres[k]
        p.op("dve", lambda h: h.bn_stats(stt[:, 0:6], src[:, 0:512]), rd=[src_res], wr=[rr[0]])
        p.op("dve", lambda h: h.bn_stats(stt[:, 6:12], src[:, 512:1024]), rd=[src_res], wr=[rr[1]])
        p.op("dve", lambda h: h.bn_aggr(mv[:, 0:2], stt[:, 0:12]), rd=[rr[0], rr[1]], wr=[rr[2]])
        p.op("act", lambda h: h.activation(rs[:, 1:2], mv[:, 1:2], AF.Ln, bias=epsc[:, eps_col:eps_col + 1]), rd=[rr[2], r_epsc], wr=[rr[3]])
        p.op("act", lambda h: h.activation(rs[:, 0:1], rs[:, 1:2], AF.Exp, scale=-0.5), rd=[rr[3]], wr=[rr[3]])
        p.op("dve", lambda h: h.scalar_tensor_tensor(nm[:, 0:1], mv[:, 0:1], -1.0, rs[:, 0:1], ALU.mult, ALU.mult),
             rd=[rr[2], rr[3]], wr=[rr[4]])
        return rs[:, 0:1], nm[:, 0:1], [rr[3], rr[4]]

    lnres = [[R("ln%d_%d" % (k, q)) for q in range(5)] for k in range(NSC)]
    r_epsc = R("epsc")
    if not os.environ.get("K_NOMEMSET"):
        p.op("dve", lambda h: h.memset(epsc, EPS), wr=[r_epsc])
        p.op("dve", lambda h: h.memset(epsc[:, 1:2], EPS / (ALPHA * ALPHA)), wr=[r_epsc])

    c_res = {}

    _lim = int(os.environ.get("K_CONST_LIMIT", "1000"))

    def load_const(name, dst, src, cast=False):
        r = R(name)
        c_res[name] = r
        if len(c_res) > _lim:
            return r
        eng = "pool" if cast else "sp"
        p.op(eng, lambda h: h.dma_start(out=dst, in_=src), wr=[r], dma=r)
        return r

    r_cT = load_const("cT", cT, cT_d)
    r_badaT = load_const("badaT", badaT, badaT_d)
    r_identb = load_const("identb", identb, identf_d, cast=True)
    r_tri = load_const("tri", trib, tri_d, cast=True)
    r_ones = load_const("ones", onesb, ones_d, cast=True)
    r_onesn = load_const("onesn", onesn, onesn_d, cast=True)
    r_mask = load_const("mask", maskb, mask_d.rearrange("a p n -> p a n"), cast=True)
    r_kbias = load_const("kbias", kbias[:, 0:NT], kbias_d)
    r_kpad = load_const("kpad", kpadb, kpad_d, cast=True)
    r_valid = load_const("valid", valid[:, 0:1], valid_d)
    r_wpool = load_const("wpool", wpool, wpool_d.rearrange("g c e -> c g e"), cast=True)
    r_wr = load_const("wr", wr, wr_d.rearrange("(c p) n -> p c n", p=128))
    r_brb = load_const("brb", brb, brb_d)
    r_wrs = R("wrs")
    p.op("dve", lambda h: h.tensor_copy(wrh, wr), rd=[r_wr], wr=[r_wrs])
    p.op("dve", lambda h: h.tensor_tensor(wrl, wr, wrh, ALU.subtract), rd=[r_wr, r_wrs], wr=[r_wrs])
    r_pscT = load_const("pscT", pscT, pscT_d)
    r_g1b = R("g1b")
    r_g2b = R("g2b")
    if not os.environ.get("K_NOG"):
        p.op("sp", lambda h: h.dma_start(out=g1b, in_=brow_d[0]), wr=[r_g1b], dma=r_g1b)
        p.op("sp", lambda h: h.dma_start(out=g2b, in_=brow_d[1]), wr=[r_g2b], dma=r_g2b)
    if phases < 0:
        return _finish_debug(nc, p, st, out_d, locals())
    r_win = [R("winQ"), R("winK"), R("winV"), R("winP")]
    r_wab = [R("wab0"), R("wab1")]
    r_scT = R("scT")
    r_modT = R("modT1")
    r_modT2 = R("modT2")
    r_screp = R("screp")
    screp, _ = carve(W0 + 29056, BF16, [128, 8, 128])
    assert W0 + 29056 + 2048 <= WEND

    p.op("act", lambda h: h.activation(scT, cT, AF.Silu), rd=[r_cT], wr=[r_scT])
    for c in range(8):
        p.op("dve", lambda h, c=c: h.tensor_scalar(screp[:, c, :], onesb, scT[:, c:c + 1], None, ALU.mult),
             rd=[r_ones, r_scT], wr=[r_screp])
    wada_v = wada_d.rearrange("(c p) n -> p c n", p=128)
    win_v = win_d.rearrange("(c p) n -> p c n", p=128)
    ada_ctr = [0]
    ada_buf = {}

    def ada_load(cb):
        b = ada_ctr[0] % 2
        ada_ctr[0] += 1
        ada_buf[cb] = b
        p.op("pool", lambda h: h.dma_start(out=wab[b], in_=wada_v[:, :, 512 * cb: 512 * cb + 512]),
             wr=[r_wab[b]], dma=r_wab[b])

    def ada_compute(cb):
        b = ada_buf[cb]
        if cb in (4, 5, 10, 11):
            dst = g1b if cb in (4, 5) else g2b
            rdst = r_g1b if cb in (4, 5) else r_g2b
            half = cb % 2
            for c in range(8):
                p.op("pe", lambda h, c=c: h.matmul(bank(0), screp[:, c, :], wab[b][:, c, :], start=(c == 0), stop=(c == 7)),
                     rd=[r_screp, r_wab[b]], wr=[r_ps[0]])
            dsl = dst[:, 512 * half: 512 * half + 512]
            p.op("dve", lambda h: h.scalar_tensor_tensor(dsl, bank(0), 1.0, dsl, ALU.add, ALU.add),
                 rd=[r_ps[0]], wr=[rdst])
        else:
            vec = {0: 0, 1: 0, 2: 1, 3: 1, 6: 2, 7: 2, 8: 3, 9: 3}[cb]
            half = cb % 2
            for sub in range(4):
                for c in range(8):
                    p.op("pe", lambda h, c=c, sub=sub: h.matmul(bank(0, sub, sub + 1), wab[b][:, c, 128 * sub:128 * sub + 128],
                                                                scT[:, c:c + 1], start=(c == 0), stop=(c == 7)),
                         rd=[r_scT, r_wab[b]], wr=[r_ps[0]])
            add1 = 1.0 if vec in (1, 3) else 0.0
            p.op("dve", lambda h: h.scalar_tensor_tensor(
                modT[:, vec, 4 * half:4 * half + 4], bank(0, 0, 4), add1, badaT[:, 4 * cb:4 * cb + 4], ALU.add, ALU.add),
                rd=[r_ps[0], r_badaT], wr=[r_modT if vec < 2 else r_modT2])

    ada_load(0)
    ada_load(1)
    ada_compute(0)
    ada_load(2)
    ada_compute(1)
    ada_load(3)
    for kb_ in (1, 2, 3, 0):
        p.op("pool", lambda h, kb_=kb_: h.dma_start(out=win[:, :, 512 * kb_:512 * kb_ + 512], in_=win_v[:, :, 512 * kb_:512 * kb_ + 512]),
             wr=[r_win[kb_]], dma=r_win[kb_])
    ada_compute(2)
    ada_compute(3)
    ada_left = [4, 5, 6, 7, 8, 9, 10, 11]
    ada_per_group = (len(ada_left) + NSLOT - 1) // NSLOT

    if phases < 1:
        return _finish_debug(nc, p, st, out_d, locals())
    r_xg = [R("xg0"), R("xg1")]
    r_xn2 = [[R("xn%d_%d" % (k, t)) for t in range(4)] for k in range(2)]
    r_uT = [R("uT%d" % c) for c in range(8)]
    _psTb = [r_ps[0]] + [R("psTb%d" % c) for c in range(1, 4)]
    r_psT = [_psTb[c // 2] for c in range(8)]
    r_pj = [R("pj%d" % k) for k in range(4)]
    r_KT = [[R("KT%d_%d" % (i, hp)) for hp in range(4)] for i in range(NSLOT)]
    r_Vt = [[R("Vt%d_%d" % (i, t)) for t in range(4)] for i in range(NSLOT)]
    r_QT = [[R("QT%d_%d" % (i, hp)) for hp in range(4)] for i in range(NSLOT)]
    r_OP = [[R("OP%d_%d" % (i, g)) for g in range(4)] for i in range(NSLOT)]
    r_OS = [[R("OS%d_%d" % (i, hp)) for hp in range(4)] for i in range(NSLOT)]
    o = W0
    xgb = []
    for k in range(2):
        t, o = carve(o, F32, [128, D]); xgb.append(t)
    PTW = 272
    pt, o = carve(o, F32, [128, 4, PTW])
    ptmp = []
    for k in range(2):
        t, o = carve(o, F32, [128, 272]); ptmp.append(t)
    pooled, o = carve(o, BF16, [128, 4, 256])
    invc, o = carve(o, F32, [128, 4, 256])
    xn_b, o = carve(o, BF16, [128, 4, D])
    assert o <= W0 + 29056, o
    xnb = [xn, xn_b]
    r_pt = [R("pt%d" % g) for g in range(4)]
    r_ptmp = [R("ptmp0"), R("ptmp1")]
    r_pooled = [R("pooled%d" % g) for g in range(4)]
    r_invc = R("invc")
    p.op("sp", lambda h: h.dma_start(out=invc, in_=invc_d), wr=[r_invc], dma=r_invc)

    pjc = [0]
    evc = [0]

    def pj_bank():
        k = pjc[0] % 4
        pjc[0] += 1
        return 4 + k, r_pj[k]

    def evac(dst, src, rsrc, rdst, scale=None, eng=None):
        if eng is None:
            e = 1 if evc[0] % 3 == 2 else 0
            evc[0] += 1
        else:
            e = eng
        rd = list(rsrc)
        if e == 0:
            if scale is None:
                p.op("act", lambda h: h.activation(dst, src, AF.Copy), rd=rd, wr=rdst)
            else:
                sap, sres = scale
                p.op("act", lambda h: h.activation(dst, src, AF.Identity, scale=sap), rd=rd + [sres], wr=rdst)
        else:
            if scale is None:
                p.op("dve", lambda h: h.tensor_copy(dst, src), rd=rd, wr=rdst)
            else:
                sap, sres = scale
                p.op("dve", lambda h: h.tensor_scalar(dst, src, sap, None, ALU.mult), rd=rd + [sres], wr=rdst)

    gtc = [0]

    def P1_LN(i, ts=(0, 1, 2, 3)):
        xnk = xnb[i % 2]
        for t in ts:
            xb = xgb[gtc[0] % 2]
            rx = r_xg[gtc[0] % 2]
            gtc[0] += 1
            row = 512 * i + 128 * t
            p.op("sp", lambda h, xb=xb, row=row: h.dma_start(out=xb, in_=xv[row:row + 128, :]), wr=[rx], dma=rx)
            rs, nm, rln = ln_stats(xb, rx, EPS)
            p.op("dve", lambda h, xb=xb, rs=rs, nm=nm, t=t: h.tensor_scalar(xnk[:, t, :], xb, rs, nm, ALU.mult, ALU.add),
                 rd=[rx] + rln, wr=[r_xn2[i % 2][t]])

    def P1_TR(i):
        xnk = xnb[i % 2]
        for c in range(8):
            for t in range(4):
                p.op("pe", lambda h, c=c, t=t: h.transpose(ps_bf[:, 512 * c + 128 * t: 512 * c + 128 * t + 128],
                                                         xnk[:, t, 128 * c:128 * c + 128], identb),
                     rd=[r_xn2[i % 2][t], r_identb], wr=[r_psT[c]])
            if c % 2 == 1:
                for c2 in (c - 1, c):
                    p.op("act", lambda h, c=c2: h.activation(uT[:, c, :], ps_bf[:, 512 * c:512 * c + 512], AF.Identity,
                                                           bias=modT[:, 0, c:c + 1], scale=modT[:, 1, c:c + 1]),
                         rd=[r_psT[c2], r_modT], wr=[r_uT[c2]])

    def P1_K(i):
        for hp in range(4):
            bk, rb = pj_bank()
            for c in range(8):
                p.op("pe", lambda h, c=c, hp=hp, bk=bk: h.matmul(bank(bk), win[:, c, 512 + 128 * hp:512 + 128 * hp + 128], uT[:, c, :],
                                                               start=(c == 0), stop=(c == 7)),
                     rd=[r_win[1], r_uT[c]], wr=[rb])
            evac(KT[:, hp, 512 * i:512 * i + 512], bank(bk), [rb], [r_KT[i][hp]], eng=0)

    def P1_V(i):
        for t in range(4):
            bk, rb = pj_bank()
            for c in range(8):
                p.op("pe", lambda h, c=c, t=t, bk=bk: h.matmul(bank(bk), uT[:, c, 128 * t:128 * t + 128], win[:, c, 1024:1536],
                                                             start=(c == 0), stop=(c == 7)),
                     rd=[r_win[2], r_uT[c]], wr=[rb])
            evac(Vt[:, 4 * i + t, :], bank(bk), [rb], [r_Vt[i][t]], eng=0)

    def P1_P(i):
        for g in range(4):
            bk, rb = pj_bank()
            for c in range(8):
                p.op("pe", lambda h, c=c, g=g, bk=bk: h.matmul(bank(bk, 128, 512), win[:, c, 1536 + 128 * g:1536 + 128 * g + 128],
                                                             uT[:, c, 128:512], start=(c == 0), stop=(c == 7)),
                     rd=[r_win[3], r_uT[c]], wr=[rb])
            evac(pt[:, g, :], bank(bk, 240, 512), [rb], [r_pt[g]], eng=1)

    def P1_Q(i):
        for hp in range(4):
            bk, rb = pj_bank()
            for c in range(8):
                p.op("pe", lambda h, c=c, hp=hp, bk=bk: h.matmul(bank(bk, 0, 256), win[:, c, 128 * hp:128 * hp + 128], uT[:, c, 256:512],
                                                               start=(c == 0), stop=(c == 7)),
                     rd=[r_win[0], r_uT[c]], wr=[rb])
            evac(QT[:, hp, 256 * i:256 * i + 256], bank(bk, 0, 256), [rb], [r_QT[i][hp]], eng=0)

    def P1_POOL(i):
        for g in range(4):
            w = 2 << g
            if i == 0:
                p.op("pool", lambda h, g=g: h.tensor_scalar(pt[:, g, 0:16], pt[:, g, 0:16], valid[:, 0:1], None, ALU.mult),
                     rd=[r_valid], wr=[r_pt[g]])
            base = 240
            cur = pt[:, g, :]
            cur_off = 240
            rcur = r_pt[g]
            for k in range(g + 1):
                sh = 1 << k
                nb = base + sh
                n = 512 - nb
                dst = ptmp[k % 2]
                rdst = r_ptmp[k % 2]
                a0 = cur[:, nb - cur_off: nb - cur_off + n]
                a1 = cur[:, nb - sh - cur_off: nb - sh - cur_off + n]
                p.op("pool", lambda h, dst=dst, a0=a0, a1=a1, n=n: h.tensor_tensor(dst[:, 0:n], a0, a1, ALU.add),
                     rd=[rcur], wr=[rdst])
                cur, cur_off, rcur, base = dst, nb, rdst, nb
            Wv = cur[:, 256 - cur_off: 512 - cur_off]
            pself = pt[:, g, 16:272]
            if i == 0:
                p.op("dve", lambda h, Wv=Wv, g=g: h.tensor_tensor(Wv, Wv, invc[:, g, :], ALU.mult),
                     rd=[r_invc], wr=[rcur])
                p.op("dve", lambda h, Wv=Wv, g=g, pself=pself: h.tensor_tensor(pooled[:, g, :], Wv, pself, ALU.subtract),
                     rd=[rcur, r_pt[g]], wr=[r_pooled[g]])
            else:
                p.op("dve", lambda h, Wv=Wv, g=g, pself=pself, w=w: h.scalar_tensor_tensor(pooled[:, g, :], Wv, 1.0 / w, pself,
                                                                                       ALU.mult, ALU.subtract),
                     rd=[rcur, r_pt[g]], wr=[r_pooled[g]])

    def P1_POOLMM(i):
        for g in range(4):
            bk, rb = pj_bank()
            p.op("pe", lambda h, g=g, bk=bk: h.matmul(bank(bk, 0, 256), wpool[:, g, :], pooled[:, g, :], start=True, stop=True),
                 rd=[r_wpool, r_pooled[g]], wr=[rb])
            evac(OP[:, g, 256 * i:256 * i + 256], bank(bk, 0, 256), [rb], [r_OP[i][g]], scale=(pscT[:, g:g + 1], r_pscT), eng=0)

    P1_LN(0)
    for i in range(NSLOT):
        nxt = i + 1 < NSLOT
        P1_TR(i)
        mine = ada_left[i * ada_per_group:(i + 1) * ada_per_group]
        for cb in mine[:2]:
            ada_load(cb)
        if nxt:
            P1_LN(i + 1, (0,))
        P1_K(i)
        if nxt:
            P1_LN(i + 1, (1,))
        if i > 0:
            P1_POOLMM(i - 1)
        P1_V(i)
        if nxt:
            P1_LN(i + 1, (2,))
        P1_P(i)
        if nxt:
            P1_LN(i + 1, (3,))
        P1_Q(i)
        for k_, cb in enumerate(mine):
            if k_ >= 2:
                ada_load(cb)
            ada_compute(cb)
        P1_POOL(i)
    P1_POOLMM(NSLOT - 1)
    r_xn = r_xn2[0] + r_xn2[1]
    for row_ in r_OS:
        for r in row_:
            p.alias(r, r_wab)

    if phases < 2:
        return _finish_debug(nc, p, st, out_d, locals())

    r_wout = R("wout")
    p.alias(r_wout, r_xn + r_uT)
    wout_v = wout_d.rearrange("(c p) n -> p c n", p=128)
    p.op("pool", lambda h: h.dma_start(out=wout, in_=wout_v), wr=[r_wout], dma=r_wout)

    o = W0
    e32, sp_bf, att, S_bf = [], [], [], []
    for k in range(3):
        t, o = carve(o, BF16, [128, 512]); e32.append(t)
    for k in range(2):
        t, o = carve(o, BF16, [128, 512]); sp_bf.append(t)
    for k in range(2):
        t, o = carve(o, BF16, [128, 512]); att.append(t)
    for k in range(2):
        t, o = carve(o, BF16, [128, 512]); S_bf.append(t)
    Qbd = []
    for k in range(2):
        t, o = carve(o, BF16, [128, 512]); Qbd.append(t)
    assert o <= W0 + 20 * KB
    r_qbd = [R("qbd0"), R("qbd1")]
    r_e32 = [R("e32_%d" % k) for k in range(3)]
    r_sp = [R("sp%d" % k) for k in range(2)]
    r_w32 = []
    r_att = [R("att%d" % k) for k in range(2)]
    r_S = [R("S%d" % k) for k in range(2)]
    ph1_work = r_xg + r_pt + r_ptmp + r_pooled + [r_invc, r_screp] + r_xn2[1]
    for r in r_e32 + r_sp + r_att + r_S + r_qbd:
        p.alias(r, ph1_work)
    for k in range(2):
        p.op("pool", lambda h, k=k: h.memset(Qbd[k], 0.0), wr=[r_qbd[k]])
    ZB = [0, 1, 2, 3, 6]
    NZ = len(ZB)
    OB = [4, 5]
    r_z = [R("z%d" % k) for k in range(NZ)]
    r_c = []
    r_o = [R("o0"), R("o1")]
    for r in r_z + r_o:
        p.alias(r, r_psT + r_pj)

    tiles = []
    seq = 0
    for i in range(NSLOT):
        for hp in range(4):
            kts = list(range(4 * i + 3, -1, -1))
            for q, kt in enumerate(kts):
                tiles.append(dict(i=i, hp=hp, kt=kt, first=(q == 0), last=(q == len(kts) - 1), seq=seq, q=q))
            seq += 1
    NTL = len(tiles)

    seqs = [(i_, hp_) for i_ in range(NSLOT) for hp_ in range(4)]

    def build_qbd(sq):
        i_, hp_ = seqs[sq]
        for hh in range(2):
            p.op("pool", lambda h, hh=hh: h.tensor_copy(Qbd[sq % 2][64 * hh:64 * hh + 64, 256 * hh:256 * hh + 256],
                                                       QT[64 * hh:64 * hh + 64, hp_, 256 * i_:256 * i_ + 256]),
                 rd=[r_QT[i_][hp_]], wr=[r_qbd[sq % 2]])

    def stA(n):
        T = tiles[n]
        i, hp, kt = T["i"], T["hp"], T["kt"]
        zb = ZB[n % NZ]
        diag = kt >= 4 * i + 2
        qk = T["seq"] % 2
        if T["first"] and T["seq"] == 0:
            build_qbd(0)
        if T["q"] == 1 and T["seq"] + 1 < len(seqs):
            build_qbd(T["seq"] + 1)
        padk = (kt < 2) and not diag
        p.op("pe", lambda h: h.matmul(bank(zb), KT[:, hp, 128 * kt:128 * kt + 128], Qbd[qk], start=True, stop=(not diag and not padk)),
             rd=[r_KT[kt // 4][hp], r_qbd[qk]], wr=[r_z[n % NZ]])
        if padk:
            p.op("pe", lambda h: h.matmul(bank(zb), identb, kpadb, start=False, stop=True),
                 rd=[r_identb, r_kpad], wr=[r_z[n % NZ]])
        if diag:
            mk = kt - (4 * i + 2)
            p.op("pe", lambda h: h.matmul(bank(zb), identb, maskb[:, mk, :], start=False, stop=True),
                 rd=[r_identb, r_mask], wr=[r_z[n % NZ]])

    def stB1(n):
        zb = ZB[n % NZ]
        eb = e32[n % 3]
        p.op("act", lambda h: h.activation(eb, bank(zb), AF.Exp, scale=0.125),
             rd=[r_z[n % NZ]], wr=[r_e32[n % 3]])

    def stB2(n):
        eb = e32[n % 3]
        p.op("act", lambda h: h.activation(sp_bf[n % 2], eb, AF.Ln, bias=1.0),
             rd=[r_e32[n % 3]], wr=[r_sp[n % 2]])

    def stC(n):
        T = tiles[n]
        zb = ZB[n % NZ]
        first = T["first"]
        q = T["q"]
        p.op("pe", lambda h: h.matmul(bank(zb), trib, sp_bf[n % 2], start=False, stop=first),
             rd=[r_tri, r_sp[n % 2]], wr=[r_z[n % NZ]])
        if not first:
            p.op("pe", lambda h: h.matmul(bank(zb), onesn, S_bf[(q - 1) % 2], start=False, stop=True),
                 rd=[r_onesn, r_S[(q - 1) % 2]], wr=[r_z[n % NZ]])
        if not T["last"]:
            if first:
                p.op("pool", lambda h: h.tensor_copy(S_bf[q % 2], sp_bf[n % 2]), rd=[r_sp[n % 2]], wr=[r_S[q % 2]])
            else:
                p.op("pool", lambda h: h.tensor_tensor(S_bf[q % 2], S_bf[(q - 1) % 2], sp_bf[n % 2], ALU.add),
                     rd=[r_sp[n % 2], r_S[(q - 1) % 2]], wr=[r_S[q % 2]])

    def stD(n):
        T = tiles[n]
        kt = T["kt"]
        zb = ZB[n % NZ]
        p.op("act", lambda h: h.activation(att[n % 2], bank(zb), AF.Exp, scale=0.125),
             rd=[r_z[n % NZ]], wr=[r_att[n % 2]])

    def stF(n):
        T = tiles[n]
        i, hp, kt = T["i"], T["hp"], T["kt"]
        ob = OB[T["seq"] % 2]
        ro = r_o[T["seq"] % 2]
        for hh in range(2):
            p.op("pe", lambda h, hh=hh: h.matmul(ps[64 * hh:64 * hh + 64, 512 * ob:512 * ob + 256],
                                                 Vt[:, kt, 128 * hp + 64 * hh:128 * hp + 64 * hh + 64],
                                                 att[n % 2][:, 256 * hh:256 * hh + 256],
                                                 start=T["first"], stop=T["last"]),
                 rd=[r_Vt[kt // 4][kt % 4], r_att[n % 2]], wr=[ro])
        if T["last"]:
            evac(OS[:, hp, 256 * i:256 * i + 256], bank(ob, 0, 256), [ro], [r_OS[i][hp]])

    stA(0)
    stA(1)
    stB1(0)
    fold_at = {min(NTL - 8 + k, 40 + 12 * k): k for k in range(8)}
    for n in range(NTL + 2):
        if n in fold_at:
            p.op("pool", lambda h, k=fold_at[n]: h.tensor_tensor(wout[:, k, :], wout[:, k, :], g1b, ALU.mult), rd=[r_g1b], wr=[r_wout])
        if 0 <= n - 1 < NTL:
            stC(n - 1)
        if n + 2 < NTL:
            stA(n + 2)
        if n + 1 < NTL:
            stB1(n + 1)
        if n < NTL:
            stB2(n)
        if 0 <= n - 1 < NTL:
            stD(n - 1)
        if 0 <= n - 2 < NTL:
            stF(n - 2)

    if phases < 3:
        return _finish_debug(nc, p, st, out_d, locals())

    ph2_work = r_e32 + r_sp + r_w32 + r_att + r_S + r_qbd
    r_acc = [R("acc%d" % T) for T in range(NOT)]
    for r in r_acc:
        p.alias(r, [x for row in r_KT for x in row] + [x for row in r_Vt for x in row])
    r_u2T = [R("u2T%d" % T) for T in range(NOT)]
    for r in r_u2T:
        p.alias(r, r_win)
    r_comb = R("comb")
    NSET = 4
    set_off = [W0, W0 + 8 * KB, W0 + 16 * KB, R2]
    xrb, xhs, xls = [], [], []
    for k in range(NSET):
        o = set_off[k]
        t, o = carve(o, F32, [128, D]); xrb.append(t)
        t, o = carve(o, BF16, [128, D]); xhs.append(t)
        t, o = carve(o, BF16, [128, D]); xls.append(t)
    bcB3, _ = carve(W0 + 24 * KB, F32, [128, D])
    bcA3, _ = carve(R2 + 8 * KB, F32, [128, D])
    def rt3(n_inner):
        nonlocal oq
        t, oq = carve(oq, F32, [128, NOT, n_inner])
        return t
    def rt2():
        nonlocal oq
        t, oq = carve(oq, F32, [128, NOT])
        return t
    oq = W0
    Lr = rt3(20); egr = rt3(4); ogr = rt3(4); penr = rt3(4); elmr = rt3(16); o1r = rt3(16); elm2r = rt3(16); o2r = rt3(16)
    gmaxr, gsumr, gpr, m1r, m2r, ddr, edr, w1r, w2r = [rt2() for _ in range(9)]
    assert oq <= W0 + 8 * KB, oq
    r_xr = [R("xr%d" % k) for k in range(NSET)]
    r_xh = [R("xh%d" % k) for k in range(NSET)]
    r_xl = [R("xl%d" % k) for k in range(NSET)]
    r_rt = R("rt")
    qt_all = [x for row in r_QT for x in row]
    for k in range(NSET):
        for r in (r_xr[k], r_xh[k], r_xl[k]):
            p.alias(r, (ph2_work + ph1_work) if k < 3 else qt_all)
    ph3_work = r_xr[0:3] + r_xh[0:3] + r_xl[0:3] + [r_rt]
    ph3_qt = [r_xr[3], r_xh[3], r_xl[3]]
    r_mix = [R("mix%d" % k) for k in range(3)]
    r_tr = [R("tr0"), R("tr1")]
    r_lg = R("lg")
    ph3_ps = r_mix + r_tr + [r_lg]
    for r in ph3_ps:
        p.alias(r, r_z + r_c + r_o + r_pj)
    MIXB = [0, 1, 2]
    TRS = [(3, 4), (5, 7)]
    LGB = 6
    r_bcA3 = R("bcA3")
    r_bcB3 = R("bcB3")
    p.alias(r_bcA3, qt_all)
    p.alias(r_bcB3, ph1_work)
    p.op("sp", lambda h: h.dma_start(out=bcA3, in_=brow_d[2]), wr=[r_bcA3], dma=r_bcA3)
    p.op("sp", lambda h: h.dma_start(out=bcB3, in_=brow_d[3]), wr=[r_bcB3], dma=r_bcB3)
    ph3_qt.append(r_bcA3)
    ph3_work.append(r_bcB3)

    def ln_parts(src, src_res, eps_col):
        k = ln_ctr[0] % NSC
        ln_ctr[0] += 1
        stt, mv, rs, nm = st_t[k], mv_t[k], rs_t[k], nm_t[k]
        rr = lnres[k]

        def f1():
            p.op("dve", lambda h: h.bn_stats(stt[:, 0:6], src[:, 0:512]), rd=[src_res], wr=[rr[0]])
            p.op("dve", lambda h: h.bn_stats(stt[:, 6:12], src[:, 512:1024]), rd=[src_res], wr=[rr[1]])
            p.op("dve", lambda h: h.bn_aggr(mv[:, 0:2], stt[:, 0:12]), rd=[rr[0], rr[1]], wr=[rr[2]])

        def f2():
            p.op("act", lambda h: h.activation(rs[:, 1:2], mv[:, 1:2], AF.Ln, bias=epsc[:, eps_col:eps_col + 1]),
                 rd=[rr[2], r_epsc], wr=[rr[3]])
            p.op("act", lambda h: h.activation(rs[:, 0:1], rs[:, 1:2], AF.Exp, scale=-0.5), rd=[rr[3]], wr=[rr[3]])

        def f3():
            p.op("dve", lambda h: h.scalar_tensor_tensor(nm[:, 0:1], mv[:, 0:1], -1.0, rs[:, 0:1], ALU.mult, ALU.mult),
                 rd=[rr[2], rr[3]], wr=[rr[4]])
        return rs[:, 0:1], nm[:, 0:1], [rr[3], rr[4]], (f1, f2, f3)

    mixc = [0]
    trc = [0]

    def tile_chain(T):
        s = T % NSET
        xr, xh, xl = xrb[s], xhs[s], xls[s]
        rxr, rxh, rxl = r_xr[s], r_xh[s], r_xl[s]
        i = T // 2
        hf = T % 2
        row = 512 * i + 256 + 128 * hf
        accT = acc[:, T, :]
        racc = r_acc[T]
        ops = []

        def add(eng, dur, fn, extra=()):
            ops.append((eng, dur, fn, extra))

        add("sp", 2.5, lambda: p.op("sp", lambda h: h.dma_start(out=xr, in_=xv[row:row + 128, :]), wr=[rxr], dma=rxr))
        for n2 in range(2):
            def f_mm(n2=n2):
                q = mixc[0] % 3
                mixc[0] += 1
                mb = MIXB[q]
                for k in range(8):
                    src = OS if k < 4 else OP
                    rsrc = (r_OS if k < 4 else r_OP)[i][k % 4]
                    p.op("pe", lambda h, k=k, src=src: h.matmul(bank(mb), src[:, k % 4, 128 * T:128 * T + 128],
                                                               wout[:, k, 512 * n2:512 * n2 + 512], start=(k == 0), stop=(k == 7)),
                         rd=[rsrc, r_wout], wr=[r_mix[q]])
                p.op("dve", lambda h: h.scalar_tensor_tensor(accT[:, 512 * n2:512 * n2 + 512], xr[:, 512 * n2:512 * n2 + 512], ALPHA,
                                                             bank(mb), ALU.mult, ALU.add),
                     rd=[rxr, r_mix[q]], wr=[racc])
            add("pe", 2.3, f_mm, [("dve", 0.7)])
        rs1, nm1, rln1, (a1, a2, a3) = ln_parts(accT, racc, 0)
        add("dve", 1.6, a1)
        add("act", 0.7, a2)
        add("dve", 0.2, a3)
        add("act", 1.15, lambda: p.op("act", lambda h: h.activation(accT, accT, AF.Identity, bias=nm1, scale=rs1), rd=rln1, wr=[racc]))
        add("dve", 1.2, lambda: p.op("dve", lambda h: h.tensor_tensor(accT, accT, bcA3, ALU.mult), rd=[r_bcA3], wr=[racc]))
        add("pool", 2.4, lambda: p.op("pool", lambda h: h.tensor_tensor(accT, accT, bcB3, ALU.add), rd=[r_bcB3], wr=[racc]))
        rs2, nm2, rln2, (b1, b2, b3) = ln_parts(accT, racc, 0)
        add("dve", 1.6, b1)
        add("act", 0.7, b2)
        add("dve", 0.2, b3)
        add("act", 1.25, lambda: p.op("act", lambda h: h.activation(xr, accT, AF.Identity, bias=nm2, scale=rs2), rd=[racc] + rln2, wr=[rxr]))
        add("act", 1.15, lambda: p.op("act", lambda h: h.activation(xh, accT, AF.Identity, bias=nm2, scale=rs2), rd=[racc] + rln2, wr=[rxh]))
        add("dve", 1.2, lambda: p.op("dve", lambda h: h.tensor_tensor(xl, xr, xh, ALU.subtract), rd=[rxr, rxh], wr=[rxl]))
        u2f = xr.rearrange("p (a b) -> p a b", b=128)
        ul = xh.rearrange("p (a b) -> p a b", b=128)

        def f_tr():
            trs = trc[0] % 2
            trc[0] += 1

            def tr_slice(c):
                bk = TRS[trs][c // 4]
                return ps[:, 512 * bk + 128 * (c % 4):512 * bk + 128 * (c % 4) + 128]
            for c in range(8):
                osl = tr_slice(c)
                p.op("pe", lambda h, c=c, osl=osl: h.matmul(osl, xh[:, 128 * c:128 * c + 128], identb, start=True, stop=False),
                     rd=[rxh, r_identb], wr=[r_tr[trs]])
                p.op("pe", lambda h, c=c, osl=osl: h.matmul(osl, xl[:, 128 * c:128 * c + 128], identb, start=False, stop=True),
                     rd=[rxl, r_identb], wr=[r_tr[trs]])
            for c in (0, 1, 2, 4, 5, 6):
                src = tr_slice(c)
                p.op("act", lambda h, c=c, src=src: h.activation(u2f[:, c, :], src, AF.Identity,
                                                                 bias=modT[:, 2, c:c + 1], scale=modT[:, 3, c:c + 1]),
                     rd=[r_tr[trs], r_modT2], wr=[rxr])
            for c in (3, 7):
                src = tr_slice(c)
                p.op("dve", lambda h, c=c, src=src: h.tensor_scalar(u2f[:, c, :], src, modT[:, 3, c:c + 1], modT[:, 2, c:c + 1],
                                                                    ALU.mult, ALU.add),
                     rd=[r_tr[trs], r_modT2], wr=[rxr])
        add("pe", 1.9, f_tr, [("act", 3.4), ("dve", 0.9)])
        add("act", 1.15, lambda: p.op("act", lambda h: h.activation(u2T[:, :, 128 * T:128 * T + 128], u2f, AF.Copy), rd=[rxr], wr=[r_u2T[T]]))
        add("dve", 1.25, lambda: p.op("dve", lambda h: h.tensor_tensor(ul, u2f, u2T[:, :, 128 * T:128 * T + 128], ALU.subtract),
                                     rd=[rxr, r_u2T[T]], wr=[rxh]))

        def f_rt():
            k3 = 0
            for c in range(8):
                for (lh, rl) in ((0, wrh), (0, wrl), (1, wrh)):
                    lhs = (u2T[:, c, 128 * T:128 * T + 128] if lh == 0 else ul[:, c, :])
                    p.op("pe", lambda h, lhs=lhs, rl=rl, c=c, k3=k3: h.matmul(bank(LGB, 20 * T, 20 * T + 20), lhs, rl[:, c, :],
                                                                            start=(k3 == 0), stop=(k3 == 23)),
                         rd=[r_u2T[T], rxh, r_wrs], wr=[r_lg])
                    k3 += 1
        add("pe", 1.0, f_rt)
        _stop = int(os.environ.get("K_P3STOP", "1000"))
        return ops[:_stop]

    def list_schedule(chains, max_inflight, lat=0.6):
        n = len(chains)
        eng_free = {}
        ptr = [0] * n
        ready = [0.0] * n
        done = [0.0] * n
        active = []
        nxt = 0
        while True:
            while nxt < n and len(active) < max_inflight:
                ready[nxt] = done[nxt - max_inflight] if nxt >= max_inflight else 0.0
                active.append(nxt)
                nxt += 1
            if not active:
                break
            best = None
            for t in active:
                eng, dur, fn, extra = chains[t][ptr[t]]
                stt_ = max(eng_free.get(eng, 0.0), ready[t])
                if best is None or stt_ < best[0]:
                    best = (stt_, t)
            stt_, t = best
            eng, dur, fn, extra = chains[t][ptr[t]]
            if os.environ.get("K_P3DBG"):
                print("P3 emit tile", t, "op", ptr[t], eng, "t=%.1f" % stt_)
            fn()
            fin = stt_ + dur
            eng_free[eng] = fin
            for (e2, d2) in extra:
                st2 = max(eng_free.get(e2, 0.0), fin + lat)
                fin = st2 + d2
                eng_free[e2] = fin
            ready[t] = fin + lat
            ptr[t] += 1
            if ptr[t] == len(chains[t]):
                done[t] = ready[t]
                active.remove(t)

    list_schedule([tile_chain(T) for T in range(NOT)], NSET)

    p.alias(r_rt, r_xr + r_xh + r_xl)
    def bcl(v, n):
        return bass.AP(v.tensor, v.offset, [list(x) for x in v.ap] + [[0, n]])

    def bcm(v, n):
        a = [list(x) for x in v.ap]
        return bass.AP(v.tensor, v.offset, [a[0], [0, n]] + a[1:])

    def dv(fn, rd=(), wr_=None):
        p.op("dve", fn, rd=[r_rt] + list(rd), wr=[r_rt] if wr_ is None else wr_)

    lg3 = bank(LGB, 0, 20 * NOT).rearrange("p (a b) -> p a b", b=20)
    L4 = Lr[:, :, 0:4]
    dv(lambda h: h.tensor_tensor(Lr, lg3, bcm(brb, NOT), ALU.add), rd=[r_lg, r_brb])
    dv(lambda h: h.tensor_reduce(gmaxr, L4, AX.X, ALU.max))
    dv(lambda h: h.tensor_tensor(egr, L4, bcl(gmaxr, 4), ALU.subtract))
    p.op("act", lambda h: h.activation(egr, egr, AF.Exp), rd=[r_rt], wr=[r_rt])
    dv(lambda h: h.tensor_reduce(gsumr, egr, AX.X, ALU.add))
    dv(lambda h: h.reciprocal(gpr, gsumr))
    dv(lambda h: h.tensor_tensor(ogr, L4, bcl(gmaxr, 4), ALU.is_equal))
    dv(lambda h: h.tensor_scalar(penr, ogr, -1.0, BIG, ALU.add, ALU.mult))
    elm4 = elmr.rearrange("p a (g e) -> p a g e", e=4)
    L16 = Lr[:, :, 4:20].rearrange("p a (g e) -> p a g e", e=4)
    pen4 = bass.AP(penr.tensor, penr.offset, [list(x) for x in penr.ap] + [[0, 4]])
    dv(lambda h: h.tensor_tensor(elm4, L16, pen4, ALU.add))
    dv(lambda h: h.tensor_reduce(m1r, elmr, AX.X, ALU.max))
    dv(lambda h: h.tensor_tensor(o1r, elmr, bcl(m1r, 16), ALU.is_equal))
    dv(lambda h: h.scalar_tensor_tensor(elm2r, o1r, -BIG, elmr, ALU.mult, ALU.add))
    dv(lambda h: h.tensor_reduce(m2r, elm2r, AX.X, ALU.max))
    dv(lambda h: h.tensor_tensor(o2r, elm2r, bcl(m2r, 16), ALU.is_equal))
    dv(lambda h: h.tensor_tensor(ddr, m2r, m1r, ALU.subtract))
    p.op("act", lambda h: h.activation(edr, ddr, AF.Exp), rd=[r_rt], wr=[r_rt])
    dv(lambda h: h.tensor_scalar(w1r, edr, 1.0, None, ALU.add))
    dv(lambda h: h.reciprocal(w1r, w1r))
    dv(lambda h: h.scalar_tensor_tensor(w1r, w1r, 1.0 / ALPHA, gpr, ALU.mult, ALU.mult))
    dv(lambda h: h.tensor_tensor(w2r, edr, w1r, ALU.mult))
    dv(lambda h: h.tensor_tensor(o1r, o1r, bcl(w1r, 16), ALU.mult))
    dv(lambda h: h.tensor_tensor(o2r, o2r, bcl(w2r, 16), ALU.mult))
    dv(lambda h: h.tensor_tensor(combAll, o1r, o2r, ALU.add), wr_=[r_rt, r_comb])

    if phases < 4:
        return _finish_debug(nc, p, st, out_d, locals())

    r_wg = [R("wg0"), R("wg1")]
    r_wu = [R("wu0"), R("wu1")]
    r_wd = [R("wd0"), R("wd1")]
    olds = [x for row in r_QT for x in row] + [x for row in r_OP for x in row] + [x for row in r_OS for x in row]
    for r in r_wg + r_wu + r_wd:
        p.alias(r, olds + ph3_qt)
    o = W0
    sg, hT = [], []
    for k in range(2):
        t, o = carve(o, F32, [128, 512]); sg.append(t)
    for k in range(2):
        t, o = carve(o, BF16, [128, 4, 512]); hT.append(t)
    assert o <= WEND
    r_sg = [R("sg0"), R("sg1")]
    r_hT = [[R("hT%d_%d" % (k, hc)) for hc in range(4)] for k in range(2)]
    for r in r_sg + [x for row in r_hT for x in row]:
        p.alias(r, ph3_work + ph2_work + ph1_work)
    r_gb = [R("gb0"), R("gb1")]
    r_ub = [R("ub0"), R("ub1")]
    r_yb = [R("yb%d" % k) for k in range(4)]
    for r in r_gb + r_ub + r_yb:
        p.alias(r, ph3_ps)
    GB = [0, 1]
    UB = [2, 3]
    YB = [4, 5, 6, 7]

    r_bcA = R("bcA5")
    r_bcB = R("bcB5")
    for r in (r_bcA, r_bcB):
        p.alias(r, ph3_work + ph2_work + ph1_work)
    p.op("sp", lambda h: h.dma_start(out=bcA, in_=brow_d[4]), wr=[r_bcA], dma=r_bcA)
    p.op("sp", lambda h: h.dma_start(out=bcB, in_=brow_d[5]), wr=[r_bcB], dma=r_bcB)
    o = W0 + 12 * KB
    otb = []
    for k in range(2):
        t, o = carve(o, F32, [128, D]); otb.append(t)
    assert o <= W0 + 20 * KB
    r_ot = [R("ot0"), R("ot1")]
    for r in r_ot:
        p.alias(r, ph3_work + ph2_work + ph1_work)

    def final_tile(T):
        ot = otb[T % 2]
        rot = r_ot[T % 2]
        rs, nm, rln = ln_stats(acc[:, T, :], r_acc[T], EPS, eps_col=1)
        p.op("act", lambda h: h.activation(ot, acc[:, T, :], AF.Identity, bias=nm, scale=rs), rd=[r_acc[T]] + rln, wr=[rot])
        p.op("dve", lambda h: h.tensor_tensor(ot, ot, bcA, ALU.mult), rd=[r_bcA], wr=[rot])
        p.op("pool", lambda h: h.tensor_tensor(ot, ot, bcB, ALU.add), rd=[r_bcB], wr=[rot])
        p.op("sp", lambda h: h.dma_start(out=out_d[128 * T:128 * T + 128, :], in_=ot), rd=[rot], dma=rot)

    def load_expert(e):
        b = e % 2
        p.op("pool", lambda h: h.dma_start(out=wgb[b], in_=wg_d[e].rearrange("(c p) n -> p c n", p=128)),
             wr=[r_wg[b]], dma=r_wg[b])
        p.op("pool", lambda h: h.dma_start(out=wub[b], in_=wu_d[e].rearrange("(c p) n -> p c n", p=128)),
             wr=[r_wu[b]], dma=r_wu[b])
        p.op("pool", lambda h: h.dma_start(out=wdb[b], in_=wd_d[e].rearrange("(c p) n -> p c n", p=128)),
             wr=[r_wd[b]], dma=r_wd[b])
        for hc in range(4):
            p.op("pool", lambda h, hc=hc: h.tensor_tensor(wdb[b][:, hc, :], wdb[b][:, hc, :], g2b, ALU.mult),
                 rd=[r_g2b], wr=[r_wd[b]])

    load_expert(0)
    kk = 0
    yk = 0
    for e in range(NEXP):
        b = e % 2
        if e + 1 < NEXP:
            load_expert(e + 1)
        for tg in range(NTG):
            hb = (e * NTG + tg) % 2
            for hc in range(4):
                k2 = kk % 2
                kk += 1
                for c in range(8):
                    p.op("pe", lambda h, c=c, hc=hc, k2=k2, b=b, tg=tg: h.matmul(bank(GB[k2]), wgb[b][:, c, 128 * hc:128 * hc + 128],
                                                                   u2T[:, c, 512 * tg:512 * tg + 512], start=(c == 0), stop=(c == 7)),
                         rd=[r_wg[b]] + r_u2T[4 * tg:4 * tg + 4], wr=[r_gb[k2]])
                for c in range(8):
                    p.op("pe", lambda h, c=c, hc=hc, k2=k2, b=b, tg=tg: h.matmul(bank(UB[k2]), wub[b][:, c, 128 * hc:128 * hc + 128],
                                                                   u2T[:, c, 512 * tg:512 * tg + 512], start=(c == 0), stop=(c == 7)),
                         rd=[r_wu[b]] + r_u2T[4 * tg:4 * tg + 4], wr=[r_ub[k2]])
                p.op("act", lambda h, k2=k2: h.activation(sg[k2], bank(GB[k2]), AF.Silu), rd=[r_gb[k2]], wr=[r_sg[k2]])
                p.op("dve", lambda h, k2=k2, hc=hc, hb=hb: h.tensor_tensor(hT[hb][:, hc, :], sg[k2], bank(UB[k2]), ALU.mult),
                     rd=[r_sg[k2], r_ub[k2]], wr=[r_hT[hb][hc]])
            for tt in range(4):
                T = 4 * tg + tt
                for n2 in range(2):
                    y2 = yk % 4
                    yk += 1
                    for hc in range(4):
                        p.op("pe", lambda h, hc=hc, tt=tt, n2=n2, y2=y2, hb=hb, b=b: h.matmul(
                            bank(YB[y2]), hT[hb][:, hc, 128 * tt:128 * tt + 128], wdb[b][:, hc, 512 * n2:512 * n2 + 512],
                            start=(hc == 0), stop=(hc == 3)),
                            rd=[r_hT[hb][hc], r_wd[b]], wr=[r_yb[y2]])
                    asl = acc[:, T, 512 * n2:512 * n2 + 512]
                    p.op("dve", lambda h, asl=asl, y2=y2, T=T, e=e: h.scalar_tensor_tensor(
                        asl, bank(YB[y2]), combAll[:, T, e:e + 1], asl, ALU.mult, ALU.add),
                        rd=[r_yb[y2], r_comb], wr=[r_acc[T]])
                if e == NEXP - 1:
                    final_tile(T)

    p.op("sp", None, wr=r_ot)
    p.emit(nc)
    st.close()
    return nc


def _finish_debug(nc, p, st, out_d, L):
    dbg = L.get("_dbg")
    p.emit(nc)
    st.close()
    return nc


def _host_inputs(NSLOT, xb, cb, j, w):
    NV = 512 * NSLOT
    NT = 4 * NSLOT
    S = xb.shape[0]
    assert S == NV
    if j == 1:
        xvirt = np.ascontiguousarray(xb)
    else:
        xvirt = np.concatenate([np.zeros((256, D), np.float32), xb[:NV - 256]], axis=0)
    kb = np.zeros((128, NT), np.float32)
    if j == 0:
        kb[:, 0:2] = NEG
    valid = np.full((128, 1), 1.0 if j == 1 else 0.0, np.float32)
    kpad = np.full((128, 512), NEG if j == 0 else 0.0, np.float32)
    invc = np.zeros((128, 4, 256), np.float32)
    pos = np.arange(256) + 256 * j
    for g in range(4):
        wv = 2 << g
        invc[:, g, :] = (1.0 / np.minimum(pos + 1, wv)).astype(np.float32)[None, :]
    m = dict(w)
    m.update(xv=xvirt, cT=np.ascontiguousarray(cb.reshape(8, 128).T), kbias=kb, kpad=kpad, valid=valid, invc=invc)
    return m


def _shared_inputs(inp):
    f = np.float32
    b_ada = inp["b_ada"][0]
    bc = lambda v: np.ascontiguousarray(np.broadcast_to(np.asarray(v, f)[None, :], (128, D)))
    brow = np.stack([bc(b_ada[2 * D:3 * D]), bc(b_ada[5 * D:6 * D]), bc(inp["ln1_g"][0]), bc(inp["ln1_b"][0]),
                     bc(inp["ln2_g"][0]), bc(inp["ln2_b"][0])], axis=0)
    k = np.arange(128)[:, None]
    q = np.arange(128)[None, :]
    tri = np.where(k >= q, -8.0, 0.0).astype(f)
    ql = np.arange(256)[None, :]
    m0 = np.where(k < ql, 0.0, NEG).astype(f)
    m1 = np.where(k + 128 < ql, 0.0, NEG).astype(f)
    maskb = np.stack([np.concatenate([m0, m0], 1), np.concatenate([m1, m1], 1)], 0)
    w = dict(
        w_ada=np.ascontiguousarray(inp["w_ada"][0]), badaT=np.ascontiguousarray(b_ada.reshape(48, 128).T),
        brow=brow, w_in=np.ascontiguousarray(inp["w_in"][0]), w_pool=np.ascontiguousarray(inp["w_pool"][0]),
        pscT=np.ascontiguousarray(inp["pool_scale"][0].reshape(4, 128).T), w_out=np.ascontiguousarray(inp["w_out"][0]),
        wr=np.ascontiguousarray(np.concatenate([inp["w_router_group"][0], inp["w_router_expert"][0]], axis=1)),
        brb=np.ascontiguousarray(np.broadcast_to(np.concatenate([inp["b_router_group"][0], inp["b_router_expert"][0]])[None, :], (128, 20))),
        w_gate=np.ascontiguousarray(inp["w_gate"][0]), w_up=np.ascontiguousarray(inp["w_up"][0]),
        w_down=np.ascontiguousarray(inp["w_down"][0]),
        identf=np.eye(128, dtype=f), tri=tri, ones=np.ones((128, 128), f), onesn=np.full((128, 128), -8.0, f), maskb=maskb,
    )
    return {k_: np.asarray(v, f) for k_, v in w.items()}


_NC_CACHE = {}


def run_cores(inp, NSLOT, phases=5):
    x = np.asarray(inp["x"], np.float32)
    c = np.asarray(inp["c"], np.float32)
    B, S, _ = x.shape
    w = _shared_inputs(inp)
    in_maps = []
    for b in range(B):
        for j in range(2):
            in_maps.append(_host_inputs(NSLOT, x[b], c[b], j, w))
    key = (NSLOT, phases)
    if key not in _NC_CACHE:
        _NC_CACHE[key] = build(NSLOT, phases)
    nc = _NC_CACHE[key]
    res = run_bass_kernel_spmd(nc, in_maps, core_ids=list(range(2 * B)))
    out = np.zeros((B, S, D), np.float32)
    for b in range(B):
        for j in range(2):
            o = np.asarray(res.results[2 * b + j]["out"])
            for i in range(NSLOT):
                out[b, 256 * (2 * i + j):256 * (2 * i + j) + 256] = o[256 * i:256 * i + 256]
    return out


def kernel(**inputs):
    return run_cores(inputs, 8)
```

```python
import os
import numpy as np
from contextlib import ExitStack
import concourse.bass as bass
import concourse.mybir as mybir
from concourse.bass_utils import run_bass_kernel_spmd

F32 = mybir.dt.float32
BF16 = mybir.dt.bfloat16
AF = mybir.ActivationFunctionType
ALU = mybir.AluOpType
AX = mybir.AxisListType

D = 1024
ALPHA = 2.0 ** 0.25
EPS = 1e-5
NEG = -30000.0
BIG = 1.0e9
NEXP = 16


class Res:
    __slots__ = ("name", "lw", "rd", "sem", "ndma")

    def __init__(self, name):
        self.name = name
        self.lw = None
        self.rd = {}
        self.sem = None
        self.ndma = 0


def _tok_key(t):
    return (t[0], t[1] if t[0] == "c" else id(t[1]))


class Prog:
    ENGS = ("pe", "act", "dve", "pool", "sp")

    def __init__(self):
        self.ops = {e: [] for e in self.ENGS}
        self.waited = {e: {} for e in self.ENGS}
        self.flag = {e: set() for e in self.ENGS}
        self.dma_res = []

    @staticmethod
    def _merge(dst, tok):
        k = _tok_key(tok)
        old = dst.get(k)
        if old is None or old[2] < tok[2]:
            dst[k] = tok

    def alias(self, new, olds):
        for o in olds:
            for t in o.rd.values():
                self._merge(new.rd, t)
            if o.lw is not None:
                self._merge(new.rd, o.lw)

    def op(self, eng, fn, rd=(), wr=(), dma=None):
        deps = {}
        for r in rd:
            if r.lw is not None:
                self._merge(deps, r.lw)
        for w in wr:
            if w.lw is not None:
                self._merge(deps, w.lw)
            for t in w.rd.values():
                self._merge(deps, t)
        waits = []
        wd = self.waited[eng]
        for k, t in deps.items():
            if t[0] == "c" and t[1] == eng and eng == "pe":
                continue
            if wd.get(k, -1) >= t[2]:
                continue
            wd[k] = t[2]
            waits.append(t)
            if t[0] == "c":
                self.flag[t[1]].add(t[2])
        idx = len(self.ops[eng])
        if dma is not None:
            if dma.ndma == 0:
                self.dma_res.append(dma)
            dma.ndma += 1
            tok = ("d", dma, dma.ndma)
        else:
            tok = ("c", eng, idx)
        self.ops[eng].append((fn, waits, dma))
        for r in rd:
            self._merge(r.rd, tok)
        for w in wr:
            w.lw = tok
            w.rd = {}
        return tok

    def emit(self, nc):
        with ExitStack() as st:
            sems = {e: st.enter_context(nc.semaphore("s_" + e)) for e in self.ENGS}
            for i, r in enumerate(self.dma_res):
                r.sem = st.enter_context(nc.semaphore("d%d" % i))
            val = {}
            for e in self.ENGS:
                cnt = 0
                m = {}
                for i in range(len(self.ops[e])):
                    if i in self.flag[e]:
                        cnt += 1
                        m[i] = cnt
                val[e] = m
            block = st.enter_context(nc.Block())

            def run(e, h):
                fl = self.flag[e]
                for i, (fn, waits, dma) in enumerate(self.ops[e]):
                    for t in waits:
                        if t[0] == "c":
                            h.wait_ge(sems[t[1]], val[t[1]][t[2]])
                        else:
                            h.wait_ge(t[1].sem, 16 * t[2])
                    if fn is None:
                        continue
                    ins = fn(h)
                    if dma is not None:
                        ins.then_inc(dma.sem, 16)
                    elif i in fl:
                        ins.then_inc(sems[e], 1)

            @block.tensor
            def _(h):
                run("pe", h)

            @block.scalar
            def _(h):
                run("act", h)

            @block.vector
            def _(h):
                run("dve", h)

            @block.gpsimd
            def _(h):
                run("pool", h)

            @block.sync
            def _(h):
                run("sp", h)
                for r in self.dma_res:
                    h.wait_ge(r.sem, 16 * r.ndma)


def build(NSLOT=8, phases=5):
    NV = 512 * NSLOT
    NO = 256 * NSLOT
    NT = 4 * NSLOT
    NOT = 2 * NSLOT
    NTG = NO // 512
    nc = bass.Bass("TRN2", target_bir_lowering=False)

    def din(name, shape):
        return nc.dram_tensor(name, shape, F32, kind="ExternalInput").ap()

    xv = din("xv", [NV, D])
    cT_d = din("cT", [128, 8])
    wada_d = din("w_ada", [D, 6 * D])
    badaT_d = din("badaT", [128, 48])
    brow_d = din("brow", [6, 128, D])
    win_d = din("w_in", [D, 2048])
    wpool_d = din("w_pool", [4, 128, 128])
    pscT_d = din("pscT", [128, 4])
    wout_d = din("w_out", [D, D])
    wr_d = din("wr", [D, 20])
    brb_d = din("brb", [128, 20])
    wg_d = din("w_gate", [NEXP, D, 512])
    wu_d = din("w_up", [NEXP, D, 512])
    wd_d = din("w_down", [NEXP, 512, D])
    identf_d = din("identf", [128, 128])
    tri_d = din("tri", [128, 128])
    ones_d = din("ones", [128, 128])
    onesn_d = din("onesn", [128, 128])
    mask_d = din("maskb", [2, 128, 512])
    kbias_d = din("kbias", [128, NT])
    kpad_d = din("kpad", [128, 512])
    valid_d = din("valid", [128, 1])
    invc_d = din("invc", [128, 4, 256])
    out_d = nc.dram_tensor("out", [NO, D], F32, kind="ExternalOutput").ap()

    p = Prog()
    st = ExitStack()
    ABYTES = 211968
    arena = st.enter_context(nc.sbuf_tensor("arena", [128, ABYTES // 4], F32))
    ps = st.enter_context(nc.psum_tensor("ps", [128, 4096], F32))
    a_f32 = arena
    a_bf = arena.bitcast(BF16)
    ps_bf = ps.bitcast(BF16)

    def carve(off, dt, shape):
        es = 4 if dt == F32 else 2
        assert off % 4 == 0
        n = 1
        for s in shape[1:]:
            n *= s
        base = a_f32 if dt == F32 else a_bf
        v = base[:, off // es: off // es + n]
        if len(shape) == 3:
            v = v.rearrange("p (a b) -> p a b", b=shape[2])
        return v, off + n * es

    KB = 1024
    KT, _ = carve(0, BF16, [128, 4, NV])
    Vt, _ = carve(32 * KB, BF16, [128, NT, 512])
    acc, _ = carve(0, F32, [128, NOT, D])
    R2 = 64 * KB
    QT, _ = carve(R2, BF16, [128, 4, NO])
    OP, _ = carve(R2 + 16 * KB, BF16, [128, 4, NO])
    OS, _ = carve(R2 + 32 * KB, BF16, [128, 4, NO])
    wgb, wub, wdb = [], [], []
    for b in range(2):
        o = R2 + 24 * KB * b
        t, o = carve(o, BF16, [128, 8, 512]); wgb.append(t)
        t, o = carve(o, BF16, [128, 8, 512]); wub.append(t)
        t, o = carve(o, BF16, [128, 4, D]); wdb.append(t)
    R3 = 112 * KB
    win, _ = carve(R3, BF16, [128, 8, 2048])
    u2T, _ = carve(R3, BF16, [128, 8, NO])
    R4 = 144 * KB
    wab = [carve(R2 + 32 * KB + 8 * KB * b, BF16, [128, 8, 512])[0] for b in range(2)]
    uT, _ = carve(R4, BF16, [128, 8, 512])
    xn, _ = carve(R4 + 8 * KB, BF16, [128, 4, D])
    wout, _ = carve(R4, BF16, [128, 8, D])
    o = 160 * KB
    identb, o = carve(o, BF16, [128, 128])
    trib, o = carve(o, BF16, [128, 128])
    onesb, o = carve(o, BF16, [128, 128])
    onesn, o = carve(o, BF16, [128, 128])
    maskb, o = carve(o, BF16, [128, 2, 512])
    kbias, o = carve(o, F32, [128, 32])
    valid, o = carve(o, F32, [128, 4])
    wpool, o = carve(o, BF16, [128, 4, 128])
    wr, o = carve(o, F32, [128, 8, 20])
    brb, o = carve(o, F32, [128, 20])
    wrh, o = carve(o, BF16, [128, 8, 20])
    wrl, o = carve(o, BF16, [128, 8, 20])
    pscT, o = carve(o, F32, [128, 4])
    badaT, o = carve(o, F32, [128, 48])
    cT, o = carve(o, F32, [128, 8])
    scT, o = carve(o, BF16, [128, 8])
    modT, o = carve(o, F32, [128, 4, 8])
    g1b, o = carve(o, F32, [128, D])
    g2b, o = carve(o, F32, [128, D])
    combAll, o = carve(o, F32, [128, NOT, 16])
    NSC = 8
    st_t, mv_t, rs_t, nm_t = [], [], [], []
    for k in range(NSC):
        t, o = carve(o, F32, [128, 12]); st_t.append(t)
        t, o = carve(o, F32, [128, 2]); mv_t.append(t)
        t, o = carve(o, F32, [128, 2]); rs_t.append(t)
        t, o = carve(o, F32, [128, 2]); nm_t.append(t)
    kpadb, o = carve(o, BF16, [128, 512])
    epsc, o = carve(o, F32, [128, 4])
    W0 = o
    WEND = ABYTES
    assert W0 + 28 * KB <= WEND, W0
    bcA, _ = carve(W0 + 20 * KB, F32, [128, D])
    bcB, _ = carve(W0 + 24 * KB, F32, [128, D])

    def R(n):
        return Res(n)

    r_ps = [R("ps%d" % i) for i in range(8)]

    def bank(i, lo=0, hi=512):
        return ps[:, 512 * i + lo: 512 * i + hi]

    ln_ctr = [0]

    def ln_stats(src, src_res, eps, eps_col=0):
        k = ln_ctr[0] % NSC
        ln_ctr[0] += 1
        stt, mv, rs, nm = st_t[k], mv_t[k], rs_t[k], nm_t[k]
        rr = lnres[k]
        p.op("dve", lambda h: h.bn_stats(stt[:, 0:6], src[:, 0:512]), rd=[src_res], wr=[rr[0]])
        p.op("dve", lambda h: h.bn_stats(stt[:, 6:12], src[:, 512:1024]), rd=[src_res], wr=[rr[1]])
        p.op("dve", lambda h: h.bn_aggr(mv[:, 0:2], stt[:, 0:12]), rd=[rr[0], rr[1]], wr=[rr[2]])
        p.op("act", lambda h: h.activation(rs[:, 1:2], mv[:, 1:2], AF.Ln, bias=epsc[:, eps_col:eps_col + 1]), rd=[rr[2], r_epsc], wr=[rr[3]])
        p.op("act", lambda h: h.activation(rs[:, 0:1], rs[:, 1:2], AF.Exp, scale=-0.5), rd=[rr[3]], wr=[rr[3]])
        p.op("dve", lambda h: h.scalar_tensor_tensor(nm[:, 0:1], mv[:, 0:1], -1.0, rs[:, 0:1], ALU.mult, ALU.mult),
             rd=[rr[2], rr[3]], wr=[rr[4]])
        return rs[:, 0:1], nm[:, 0:1], [rr[3], rr[4]]

    lnres = [[R("ln%d_%d" % (k, q)) for q in range(5)] for k in range(NSC)]
    r_epsc = R("epsc")
    if not os.environ.get("K_NOMEMSET"):
        p.op("dve", lambda h: h.memset(epsc, EPS), wr=[r_epsc])
        p.op("dve", lambda h: h.memset(epsc[:, 1:2], EPS / (ALPHA * ALPHA)), wr=[r_epsc])

    c_res = {}

    _lim = int(os.environ.get("K_CONST_LIMIT", "1000"))

    def load_const(name, dst, src, cast=False):
        r = R(name)
        c_res[name] = r
        if len(c_res) > _lim:
            return r
        eng = "pool" if cast else "sp"
        p.op(eng, lambda h: h.dma_start(out=dst, in_=src), wr=[r], dma=r)
        return r

    r_cT = load_const("cT", cT, cT_d)
    r_badaT = load_const("badaT", badaT, badaT_d)
    r_identb = load_const("identb", identb, identf_d, cast=True)
    r_tri = load_const("tri", trib, tri_d, cast=True)
    r_ones = load_const("ones", onesb, ones_d, cast=True)
    r_onesn = load_const("onesn", onesn, onesn_d, cast=True)
    r_mask = load_const("mask", maskb, mask_d.rearrange("a p n -> p a n"), cast=True)
    r_kbias = load_const("kbias", kbias[:, 0:NT], kbias_d)
    r_kpad = load_const("kpad", kpadb, kpad_d, cast=True)
    r_valid = load_const("valid", valid[:, 0:1], valid_d)
    r_wpool = load_const("wpool", wpool, wpool_d.rearrange("g c e -> c g e"), cast=True)
    r_wr = load_const("wr", wr, wr_d.rearrange("(c p) n -> p c n", p=128))
    r_brb = load_const("brb", brb, brb_d)
    r_wrs = R("wrs")
    p.op("dve", lambda h: h.tensor_copy(wrh, wr), rd=[r_wr], wr=[r_wrs])
    p.op("dve", lambda h: h.tensor_tensor(wrl, wr, wrh, ALU.subtract), rd=[r_wr, r_wrs], wr=[r_wrs])
    r_pscT = load_const("pscT", pscT, pscT_d)
    r_g1b = R("g1b")
    r_g2b = R("g2b")
    if not os.environ.get("K_NOG"):
        p.op("sp", lambda h: h.dma_start(out=g1b, in_=brow_d[0]), wr=[r_g1b], dma=r_g1b)
        p.op("sp", lambda h: h.dma_start(out=g2b, in_=brow_d[1]), wr=[r_g2b], dma=r_g2b)
    if phases < 0:
        return _finish_debug(nc, p, st, out_d, locals())
    r_win = [R("winQ"), R("winK"), R("winV"), R("winP")]
    r_wab = [R("wab0"), R("wab1")]
    r_scT = R("scT")
    r_modT = R("modT1")
    r_modT2 = R("modT2")
    r_screp = R("screp")
    screp, _ = carve(W0 + 29056, BF16, [128, 8, 128])
    assert W0 + 29056 + 2048 <= WEND

    p.op("act", lambda h: h.activation(scT, cT, AF.Silu), rd=[r_cT], wr=[r_scT])
    for c in range(8):
        p.op("dve", lambda h, c=c: h.tensor_scalar(screp[:, c, :], onesb, scT[:, c:c + 1], None, ALU.mult),
             rd=[r_ones, r_scT], wr=[r_screp])
    wada_v = wada_d.rearrange("(c p) n -> p c n", p=128)
    win_v = win_d.rearrange("(c p) n -> p c n", p=128)
    ada_ctr = [0]
    ada_buf = {}

    def ada_load(cb):
        b = ada_ctr[0] % 2
        ada_ctr[0] += 1
        ada_buf[cb] = b
        p.op("pool", lambda h: h.dma_start(out=wab[b], in_=wada_v[:, :, 512 * cb: 512 * cb + 512]),
             wr=[r_wab[b]], dma=r_wab[b])

    def ada_compute(cb):
        b = ada_buf[cb]
        if cb in (4, 5, 10, 11):
            dst = g1b if cb in (4, 5) else g2b
            rdst = r_g1b if cb in (4, 5) else r_g2b
            half = cb % 2
            for c in range(8):
                p.op("pe", lambda h, c=c: h.matmul(bank(0), screp[:, c, :], wab[b][:, c, :], start=(c == 0), stop=(c == 7)),
                     rd=[r_screp, r_wab[b]], wr=[r_ps[0]])
            dsl = dst[:, 512 * half: 512 * half + 512]
            p.op("dve", lambda h: h.scalar_tensor_tensor(dsl, bank(0), 1.0, dsl, ALU.add, ALU.add),
                 rd=[r_ps[0]], wr=[rdst])
        else:
            vec = {0: 0, 1: 0, 2: 1, 3: 1, 6: 2, 7: 2, 8: 3, 9: 3}[cb]
            half = cb % 2
            for sub in range(4):
                for c in range(8):
                    p.op("pe", lambda h, c=c, sub=sub: h.matmul(bank(0, sub, sub + 1), wab[b][:, c, 128 * sub:128 * sub + 128],
                                                                scT[:, c:c + 1], start=(c == 0), stop=(c == 7)),
                         rd=[r_scT, r_wab[b]], wr=[r_ps[0]])
            add1 = 1.0 if vec in (1, 3) else 0.0
            p.op("dve", lambda h: h.scalar_tensor_tensor(
                modT[:, vec, 4 * half:4 * half + 4], bank(0, 0, 4), add1, badaT[:, 4 * cb:4 * cb + 4], ALU.add, ALU.add),
                rd=[r_ps[0], r_badaT], wr=[r_modT if vec < 2 else r_modT2])

    ada_load(0)
    ada_load(1)
    ada_compute(0)
    ada_load(2)
    ada_compute(1)
    ada_load(3)
    for kb_ in (1, 2, 3, 0):
        p.op("pool", lambda h, kb_=kb_: h.dma_start(out=win[:, :, 512 * kb_:512 * kb_ + 512], in_=win_v[:, :, 512 * kb_:512 * kb_ + 512]),
             wr=[r_win[kb_]], dma=r_win[kb_])
    ada_compute(2)
    ada_compute(3)
    ada_left = [4, 5, 6, 7, 8, 9, 10, 11]
    ada_per_group = (len(ada_left) + NSLOT - 1) // NSLOT

    if phases < 1:
        return _finish_debug(nc, p, st, out_d, locals())
    r_xg = [R("xg0"), R("xg1")]
    r_xn2 = [[R("xn%d_%d" % (k, t)) for t in range(4)] for k in range(2)]
    r_uT = [R("uT%d" % c) for c in range(8)]
    _psTb = [r_ps[0]] + [R("psTb%d" % c) for c in range(1, 4)]
    r_psT = [_psTb[c // 2] for c in range(8)]
    r_pj = [R("pj%d" % k) for k in range(4)]
    r_KT = [[R("KT%d_%d" % (i, hp)) for hp in range(4)] for i in range(NSLOT)]
    r_Vt = [[R("Vt%d_%d" % (i, t)) for t in range(4)] for i in range(NSLOT)]
    r_QT = [[R("QT%d_%d" % (i, hp)) for hp in range(4)] for i in range(NSLOT)]
    r_OP = [[R("OP%d_%d" % (i, g)) for g in range(4)] for i in range(NSLOT)]
    r_OS = [[R("OS%d_%d" % (i, hp)) for hp in range(4)] for i in range(NSLOT)]
    o = W0
    xgb = []
    for k in range(2):
        t, o = carve(o, F32, [128, D]); xgb.append(t)
    PTW = 272
    pt, o = carve(o, F32, [128, 4, PTW])
    ptmp = []
    for k in range(2):
        t, o = carve(o, F32, [128, 272]); ptmp.append(t)
    pooled, o = carve(o, BF16, [128, 4, 256])
    invc, o = carve(o, F32, [128, 4, 256])
    xn_b, o = carve(o, BF16, [128, 4, D])
    assert o <= W0 + 29056, o
    xnb = [xn, xn_b]
    r_pt = [R("pt%d" % g) for g in range(4)]
    r_ptmp = [R("ptmp0"), R("ptmp1")]
    r_pooled = [R("pooled%d" % g) for g in range(4)]
    r_invc = R("invc")
    p.op("sp", lambda h: h.dma_start(out=invc, in_=invc_d), wr=[r_invc], dma=r_invc)

    pjc = [0]
    evc = [0]

    def pj_bank():
        k = pjc[0] % 4
        pjc[0] += 1
        return 4 + k, r_pj[k]

    def evac(dst, src, rsrc, rdst, scale=None, eng=None):
        if eng is None:
            e = 1 if evc[0] % 3 == 2 else 0
            evc[0] += 1
        else:
            e = eng
        rd = list(rsrc)
        if e == 0:
            if scale is None:
                p.op("act", lambda h: h.activation(dst, src, AF.Copy), rd=rd, wr=rdst)
            else:
                sap, sres = scale
                p.op("act", lambda h: h.activation(dst, src, AF.Identity, scale=sap), rd=rd + [sres], wr=rdst)
        else:
            if scale is None:
                p.op("dve", lambda h: h.tensor_copy(dst, src), rd=rd, wr=rdst)
            else:
                sap, sres = scale
                p.op("dve", lambda h: h.tensor_scalar(dst, src, sap, None, ALU.mult), rd=rd + [sres], wr=rdst)

    gtc = [0]

    def P1_LN(i, ts=(0, 1, 2, 3)):
        xnk = xnb[i % 2]
        for t in ts:
            xb = xgb[gtc[0] % 2]
            rx = r_xg[gtc[0] % 2]
            gtc[0] += 1
            row = 512 * i + 128 * t
            p.op("sp", lambda h, xb=xb, row=row: h.dma_start(out=xb, in_=xv[row:row + 128, :]), wr=[rx], dma=rx)
            rs, nm, rln = ln_stats(xb, rx, EPS)
            p.op("dve", lambda h, xb=xb, rs=rs, nm=nm, t=t: h.tensor_scalar(xnk[:, t, :], xb, rs, nm, ALU.mult, ALU.add),
                 rd=[rx] + rln, wr=[r_xn2[i % 2][t]])

    def P1_TR(i):
        xnk = xnb[i % 2]
        for c in range(8):
            for t in range(4):
                p.op("pe", lambda h, c=c, t=t: h.transpose(ps_bf[:, 512 * c + 128 * t: 512 * c + 128 * t + 128],
                                                         xnk[:, t, 128 * c:128 * c + 128], identb),
                     rd=[r_xn2[i % 2][t], r_identb], wr=[r_psT[c]])
            if c % 2 == 1:
                for c2 in (c - 1, c):
                    p.op("act", lambda h, c=c2: h.activation(uT[:, c, :], ps_bf[:, 512 * c:512 * c + 512], AF.Identity,
                                                           bias=modT[:, 0, c:c + 1], scale=modT[:, 1, c:c + 1]),
                         rd=[r_psT[c2], r_modT], wr=[r_uT[c2]])

    def P1_K(i):
        for hp in range(4):
            bk, rb = pj_bank()
            for c in range(8):
                p.op("pe", lambda h, c=c, hp=hp, bk=bk: h.matmul(bank(bk), win[:, c, 512 + 128 * hp:512 + 128 * hp + 128], uT[:, c, :],
                                                               start=(c == 0), stop=(c == 7)),
                     rd=[r_win[1], r_uT[c]], wr=[rb])
            evac(KT[:, hp, 512 * i:512 * i + 512], bank(bk), [rb], [r_KT[i][hp]], eng=0)

    def P1_V(i):
        for t in range(4):
            bk, rb = pj_bank()
            for c in range(8):
                p.op("pe", lambda h, c=c, t=t, bk=bk: h.matmul(bank(bk), uT[:, c, 128 * t:128 * t + 128], win[:, c, 1024:1536],
                                                             start=(c == 0), stop=(c == 7)),
                     rd=[r_win[2], r_uT[c]], wr=[rb])
            evac(Vt[:, 4 * i + t, :], bank(bk), [rb], [r_Vt[i][t]], eng=0)

    def P1_P(i):
        for g in range(4):
            bk, rb = pj_bank()
            for c in range(8):
                p.op("pe", lambda h, c=c, g=g, bk=bk: h.matmul(bank(bk, 128, 512), win[:, c, 1536 + 128 * g:1536 + 128 * g + 128],
                                                             uT[:, c, 128:512], start=(c == 0), stop=(c == 7)),
                     rd=[r_win[3], r_uT[c]], wr=[rb])
            evac(pt[:, g, :], bank(bk, 240, 512), [rb], [r_pt[g]], eng=1)

    def P1_Q(i):
        for hp in range(4):
            bk, rb = pj_bank()
            for c in range(8):
                p.op("pe", lambda h, c=c, hp=hp, bk=bk: h.matmul(bank(bk, 0, 256), win[:, c, 128 * hp:128 * hp + 128], uT[:, c, 256:512],
                                                               start=(c == 0), stop=(c == 7)),
                     rd=[r_win[0], r_uT[c]], wr=[rb])
            evac(QT[:, hp, 256 * i:256 * i + 256], bank(bk, 0, 256), [rb], [r_QT[i][hp]], eng=0)

    def P1_POOL(i):
        for g in range(4):
            w = 2 << g
            if i == 0:
                p.op("pool", lambda h, g=g: h.tensor_scalar(pt[:, g, 0:16], pt[:, g, 0:16], valid[:, 0:1], None, ALU.mult),
                     rd=[r_valid], wr=[r_pt[g]])
            base = 240
            cur = pt[:, g, :]
            cur_off = 240
            rcur = r_pt[g]
            for k in range(g + 1):
                sh = 1 << k
                nb = base + sh
                n = 512 - nb
                dst = ptmp[k % 2]
                rdst = r_ptmp[k % 2]
                a0 = cur[:, nb - cur_off: nb - cur_off + n]
                a1 = cur[:, nb - sh - cur_off: nb - sh - cur_off + n]
                p.op("pool", lambda h, dst=dst, a0=a0, a1=a1, n=n: h.tensor_tensor(dst[:, 0:n], a0, a1, ALU.add),
                     rd=[rcur], wr=[rdst])
                cur, cur_off, rcur, base = dst, nb, rdst, nb
            Wv = cur[:, 256 - cur_off: 512 - cur_off]
            pself = pt[:, g, 16:272]
            if i == 0:
                p.op("dve", lambda h, Wv=Wv, g=g: h.tensor_tensor(Wv, Wv, invc[:, g, :], ALU.mult),
                     rd=[r_invc], wr=[rcur])
                p.op("dve", lambda h, Wv=Wv, g=g, pself=pself: h.tensor_tensor(pooled[:, g, :], Wv, pself, ALU.subtract),
                     rd=[rcur, r_pt[g]], wr=[r_pooled[g]])
            else:
                p.op("dve", lambda h, Wv=Wv, g=g, pself=pself, w=w: h.scalar_tensor_tensor(pooled[:, g, :], Wv, 1.0 / w, pself,
                                                                                       ALU.mult, ALU.subtract),
                     rd=[rcur, r_pt[g]], wr=[r_pooled[g]])

    def P1_POOLMM(i):
        for g in range(4):
            bk, rb = pj_bank()
            p.op("pe", lambda h, g=g, bk=bk: h.matmul(bank(bk, 0, 256), wpool[:, g, :], pooled[:, g, :], start=True, stop=True),
                 rd=[r_wpool, r_pooled[g]], wr=[rb])
            evac(OP[:, g, 256 * i:256 * i + 256], bank(bk, 0, 256), [rb], [r_OP[i][g]], scale=(pscT[:, g:g + 1], r_pscT), eng=0)

    P1_LN(0)
    for i in range(NSLOT):
        nxt = i + 1 < NSLOT
        P1_TR(i)
        mine = ada_left[i * ada_per_group:(i + 1) * ada_per_group]
        for cb in mine[:2]:
            ada_load(cb)
        if nxt:
            P1_LN(i + 1, (0,))
        P1_K(i)
        if nxt:
            P1_LN(i + 1, (1,))
        if i > 0:
            P1_POOLMM(i - 1)
        P1_V(i)
        if nxt:
            P1_LN(i + 1, (2,))
        P1_P(i)
        if nxt:
            P1_LN(i + 1, (3,))
        P1_Q(i)
        for k_, cb in enumerate(mine):
            if k_ >= 2:
                ada_load(cb)
            ada_compute(cb)
        P1_POOL(i)
    P1_POOLMM(NSLOT - 1)
    r_xn = r_xn2[0] + r_xn2[1]
    for row_ in r_OS:
        for r in row_:
            p.alias(r, r_wab)

    if phases < 2:
        return _finish_debug(nc, p, st, out_d, locals())

    r_wout = R("wout")
    p.alias(r_wout, r_xn + r_uT)
    wout_v = wout_d.rearrange("(c p) n -> p c n", p=128)
    p.op("pool", lambda h: h.dma_start(out=wout, in_=wout_v), wr=[r_wout], dma=r_wout)

    o = W0
    e32, sp_bf, att, S_bf = [], [], [], []
    for k in range(3):
        t, o = carve(o, BF16, [128, 512]); e32.append(t)
    for k in range(2):
        t, o = carve(o, BF16, [128, 512]); sp_bf.append(t)
    for k in range(2):
        t, o = carve(o, BF16, [128, 512]); att.append(t)
    for k in range(2):
        t, o = carve(o, BF16, [128, 512]); S_bf.append(t)
    Qbd = []
    for k in range(3):
        t, o = carve(o, BF16, [128, 512]); Qbd.append(t)
    assert o <= W0 + 20 * KB
    r_qbd = [R("qbd0"), R("qbd1"), R("qbd2")]
    r_e32 = [R("e32_%d" % k) for k in range(3)]
    r_sp = [R("sp%d" % k) for k in range(2)]
    r_w32 = []
    r_att = [R("att%d" % k) for k in range(2)]
    r_S = [R("S%d" % k) for k in range(2)]
    ph1_work = r_xg + r_pt + r_ptmp + r_pooled + [r_invc, r_screp] + r_xn2[1]
    for r in r_e32 + r_sp + r_att + r_S + r_qbd:
        p.alias(r, ph1_work)
    for k in range(3):
        p.op("pool", lambda h, k=k: h.memset(Qbd[k], 0.0), wr=[r_qbd[k]])
    ZB = [0, 1, 6]
    NZ = len(ZB)
    BB = [2, 3, 7]
    NB = len(BB)
    OB = [4, 5]
    r_z = [R("z%d" % k) for k in range(NZ)]
    r_c = [R("b%d" % k) for k in range(NB)]
    r_o = [R("o0"), R("o1")]
    for r in r_z + r_c + r_o:
        p.alias(r, r_psT + r_pj)

    tiles = []
    seq = 0
    for i in range(NSLOT):
        for hp in range(4):
            kts = list(range(4 * i + 3, -1, -1))
            for q, kt in enumerate(kts):
                tiles.append(dict(i=i, hp=hp, kt=kt, first=(q == 0), last=(q == len(kts) - 1), seq=seq, q=q))
            seq += 1
    NTL = len(tiles)

    seqs = [(i_, hp_) for i_ in range(NSLOT) for hp_ in range(4)]

    def build_qbd(sq):
        i_, hp_ = seqs[sq]
        for hh in range(2):
            p.op("pool", lambda h, hh=hh: h.tensor_copy(Qbd[sq % 3][64 * hh:64 * hh + 64, 256 * hh:256 * hh + 256],
                                                       QT[64 * hh:64 * hh + 64, hp_, 256 * i_:256 * i_ + 256]),
                 rd=[r_QT[i_][hp_]], wr=[r_qbd[sq % 3]])

    def score_mms(n, bk, rbk, close):
        T = tiles[n]
        i, hp, kt = T["i"], T["hp"], T["kt"]
        diag = kt >= 4 * i + 2
        padk = (kt < 2) and not diag
        qk = T["seq"] % 3
        p.op("pe", lambda h: h.matmul(bank(bk), KT[:, hp, 128 * kt:128 * kt + 128], Qbd[qk], start=True,
                                      stop=(close and not diag and not padk)),
             rd=[r_KT[kt // 4][hp], r_qbd[qk]], wr=[rbk])
        if padk:
            p.op("pe", lambda h: h.matmul(bank(bk), identb, kpadb, start=False, stop=close),
                 rd=[r_identb, r_kpad], wr=[rbk])
        if diag:
            mk = kt - (4 * i + 2)
            p.op("pe", lambda h: h.matmul(bank(bk), identb, maskb[:, mk, :], start=False, stop=close),
                 rd=[r_identb, r_mask], wr=[rbk])

    def stA(n):
        T = tiles[n]
        if T["first"] and T["seq"] == 0:
            build_qbd(0)
        if T["q"] == 1 and T["seq"] + 1 < len(seqs):
            build_qbd(T["seq"] + 1)
        score_mms(n, ZB[n % NZ], r_z[n % NZ], True)

    def stB1(n):
        zb = ZB[n % NZ]
        eb = e32[n % 3]
        p.op("act", lambda h: h.activation(eb, bank(zb), AF.Exp, scale=0.125),
             rd=[r_z[n % NZ]], wr=[r_e32[n % 3]])

    def stB2(n):
        eb = e32[n % 3]
        p.op("act", lambda h: h.activation(sp_bf[n % 2], eb, AF.Ln, bias=1.0),
             rd=[r_e32[n % 3]], wr=[r_sp[n % 2]])

    def stC(n):
        T = tiles[n]
        bb = BB[n % NB]
        rbb = r_c[n % NB]
        first = T["first"]
        q = T["q"]
        score_mms(n, bb, rbb, False)
        p.op("pe", lambda h: h.matmul(bank(bb), trib, sp_bf[n % 2], start=False, stop=first),
             rd=[r_tri, r_sp[n % 2]], wr=[rbb])
        if not first:
            p.op("pe", lambda h: h.matmul(bank(bb), onesn, S_bf[(q - 1) % 2], start=False, stop=True),
                 rd=[r_onesn, r_S[(q - 1) % 2]], wr=[rbb])
        if not T["last"]:
            if first:
                p.op("pool", lambda h: h.tensor_copy(S_bf[q % 2], sp_bf[n % 2]), rd=[r_sp[n % 2]], wr=[r_S[q % 2]])
            else:
                p.op("pool", lambda h: h.tensor_tensor(S_bf[q % 2], S_bf[(q - 1) % 2], sp_bf[n % 2], ALU.add),
                     rd=[r_sp[n % 2], r_S[(q - 1) % 2]], wr=[r_S[q % 2]])

    def stD(n):
        bb = BB[n % NB]
        p.op("act", lambda h: h.activation(att[n % 2], bank(bb), AF.Exp, scale=0.125),
             rd=[r_c[n % NB]], wr=[r_att[n % 2]])

    def stF(n):
        T = tiles[n]
        i, hp, kt = T["i"], T["hp"], T["kt"]
        ob = OB[T["seq"] % 2]
        ro = r_o[T["seq"] % 2]
        for hh in range(2):
            p.op("pe", lambda h, hh=hh: h.matmul(ps[64 * hh:64 * hh + 64, 512 * ob:512 * ob + 256],
                                                 Vt[:, kt, 128 * hp + 64 * hh:128 * hp + 64 * hh + 64],
                                                 att[n % 2][:, 256 * hh:256 * hh + 256],
                                                 start=T["first"], stop=T["last"]),
                 rd=[r_Vt[kt // 4][kt % 4], r_att[n % 2]], wr=[ro])
        if T["last"]:
            evac(OS[:, hp, 256 * i:256 * i + 256], bank(ob, 0, 256), [ro], [r_OS[i][hp]])

    stA(0)
    stA(1)
    stB1(0)
    fold_at = {min(NTL - 8 + k, 40 + 12 * k): k for k in range(8)}
    for n in range(NTL + 2):
        if n in fold_at:
            p.op("pool", lambda h, k=fold_at[n]: h.tensor_tensor(wout[:, k, :], wout[:, k, :], g1b, ALU.mult), rd=[r_g1b], wr=[r_wout])
        if 0 <= n - 1 < NTL:
            stC(n - 1)
        if n + 2 < NTL:
            stA(n + 2)
        if n + 1 < NTL:
            stB1(n + 1)
        if n < NTL:
            stB2(n)
        if 0 <= n - 1 < NTL:
            stD(n - 1)
        if 0 <= n - 2 < NTL:
            stF(n - 2)

    if phases < 3:
        return _finish_debug(nc, p, st, out_d, locals())

    ph2_work = r_e32 + r_sp + r_w32 + r_att + r_S + r_qbd
    r_acc = [R("acc%d" % T) for T in range(NOT)]
    for r in r_acc:
        p.alias(r, [x for row in r_KT for x in row] + [x for row in r_Vt for x in row])
    r_u2T = [R("u2T%d" % T) for T in range(NOT)]
    for r in r_u2T:
        p.alias(r, r_win)
    r_comb = R("comb")
    NSET = 4
    set_off = [W0, W0 + 8 * KB, W0 + 16 * KB, R2]
    xrb, xhs, xls = [], [], []
    for k in range(NSET):
        o = set_off[k]
        t, o = carve(o, F32, [128, D]); xrb.append(t)
        t, o = carve(o, BF16, [128, D]); xhs.append(t)
        t, o = carve(o, BF16, [128, D]); xls.append(t)
    bcB3, _ = carve(W0 + 24 * KB, F32, [128, D])
    bcA3, _ = carve(R2 + 8 * KB, F32, [128, D])
    def rt3(n_inner):
        nonlocal oq
        t, oq = carve(oq, F32, [128, NOT, n_inner])
        return t
    def rt2():
        nonlocal oq
        t, oq = carve(oq, F32, [128, NOT])
        return t
    oq = W0
    Lr = rt3(20); egr = rt3(4); ogr = rt3(4); penr = rt3(4); elmr = rt3(16); o1r = rt3(16); elm2r = rt3(16); o2r = rt3(16)
    gmaxr, gsumr, gpr, m1r, m2r, ddr, edr, w1r, w2r = [rt2() for _ in range(9)]
    assert oq <= W0 + 8 * KB, oq
    r_xr = [R("xr%d" % k) for k in range(NSET)]
    r_xh = [R("xh%d" % k) for k in range(NSET)]
    r_xl = [R("xl%d" % k) for k in range(NSET)]
    r_rt = R("rt")
    qt_all = [x for row in r_QT for x in row]
    for k in range(NSET):
        for r in (r_xr[k], r_xh[k], r_xl[k]):
            p.alias(r, (ph2_work + ph1_work) if k < 3 else qt_all)
    ph3_work = r_xr[0:3] + r_xh[0:3] + r_xl[0:3] + [r_rt]
    ph3_qt = [r_xr[3], r_xh[3], r_xl[3]]
    r_mix = [R("mix%d" % k) for k in range(3)]
    r_tr = [R("tr0"), R("tr1")]
    r_lg = R("lg")
    ph3_ps = r_mix + r_tr + [r_lg]
    for r in ph3_ps:
        p.alias(r, r_z + r_c + r_o + r_pj)
    MIXB = [0, 1, 2]
    TRS = [(3, 4), (5, 7)]
    LGB = 6
    r_bcA3 = R("bcA3")
    r_bcB3 = R("bcB3")
    p.alias(r_bcA3, qt_all)
    p.alias(r_bcB3, ph1_work)
    p.op("sp", lambda h: h.dma_start(out=bcA3, in_=brow_d[2]), wr=[r_bcA3], dma=r_bcA3)
    p.op("sp", lambda h: h.dma_start(out=bcB3, in_=brow_d[3]), wr=[r_bcB3], dma=r_bcB3)
    ph3_qt.append(r_bcA3)
    ph3_work.append(r_bcB3)

    def ln_parts(src, src_res, eps_col):
        k = ln_ctr[0] % NSC
        ln_ctr[0] += 1
        stt, mv, rs, nm = st_t[k], mv_t[k], rs_t[k], nm_t[k]
        rr = lnres[k]

        def f1():
            p.op("dve", lambda h: h.bn_stats(stt[:, 0:6], src[:, 0:512]), rd=[src_res], wr=[rr[0]])
            p.op("dve", lambda h: h.bn_stats(stt[:, 6:12], src[:, 512:1024]), rd=[src_res], wr=[rr[1]])
            p.op("dve", lambda h: h.bn_aggr(mv[:, 0:2], stt[:, 0:12]), rd=[rr[0], rr[1]], wr=[rr[2]])

        def f2():
            p.op("act", lambda h: h.activation(rs[:, 1:2], mv[:, 1:2], AF.Ln, bias=epsc[:, eps_col:eps_col + 1]),
                 rd=[rr[2], r_epsc], wr=[rr[3]])
            p.op("act", lambda h: h.activation(rs[:, 0:1], rs[:, 1:2], AF.Exp, scale=-0.5), rd=[rr[3]], wr=[rr[3]])

        def f3():
            p.op("dve", lambda h: h.scalar_tensor_tensor(nm[:, 0:1], mv[:, 0:1], -1.0, rs[:, 0:1], ALU.mult, ALU.mult),
                 rd=[rr[2], rr[3]], wr=[rr[4]])
        return rs[:, 0:1], nm[:, 0:1], [rr[3], rr[4]], (f1, f2, f3)

    mixc = [0]
    trc = [0]

    def tile_chain(T):
        s = T % NSET
        xr, xh, xl = xrb[s], xhs[s], xls[s]
        rxr, rxh, rxl = r_xr[s], r_xh[s], r_xl[s]
        i = T // 2
        hf = T % 2
        row = 512 * i + 256 + 128 * hf
        accT = acc[:, T, :]
        racc = r_acc[T]
        ops = []

        def add(eng, dur, fn, extra=()):
            ops.append((eng, dur, fn, extra))

        add("sp", 2.5, lambda: p.op("sp", lambda h: h.dma_start(out=xr, in_=xv[row:row + 128, :]), wr=[rxr], dma=rxr))
        for n2 in range(2):
            def f_mm(n2=n2):
                q = mixc[0] % 3
                mixc[0] += 1
                mb = MIXB[q]
                for k in range(8):
                    src = OS if k < 4 else OP
                    rsrc = (r_OS if k < 4 else r_OP)[i][k % 4]
                    p.op("pe", lambda h, k=k, src=src: h.matmul(bank(mb), src[:, k % 4, 128 * T:128 * T + 128],
                                                               wout[:, k, 512 * n2:512 * n2 + 512], start=(k == 0), stop=(k == 7)),
                         rd=[rsrc, r_wout], wr=[r_mix[q]])
                p.op("dve", lambda h: h.scalar_tensor_tensor(accT[:, 512 * n2:512 * n2 + 512], xr[:, 512 * n2:512 * n2 + 512], ALPHA,
                                                             bank(mb), ALU.mult, ALU.add),
                     rd=[rxr, r_mix[q]], wr=[racc])
            add("pe", 2.3, f_mm, [("dve", 0.7)])
        rs1, nm1, rln1, (a1, a2, a3) = ln_parts(accT, racc, 0)
        add("dve", 1.6, a1)
        add("act", 0.7, a2)
        add("dve", 0.2, a3)
        add("act", 1.15, lambda: p.op("act", lambda h: h.activation(accT, accT, AF.Identity, bias=nm1, scale=rs1), rd=rln1, wr=[racc]))
        add("dve", 1.2, lambda: p.op("dve", lambda h: h.tensor_tensor(accT, accT, bcA3, ALU.mult), rd=[r_bcA3], wr=[racc]))
        add("pool", 2.4, lambda: p.op("pool", lambda h: h.tensor_tensor(accT, accT, bcB3, ALU.add), rd=[r_bcB3], wr=[racc]))
        rs2, nm2, rln2, (b1, b2, b3) = ln_parts(accT, racc, 0)
        add("dve", 1.6, b1)
        add("act", 0.7, b2)
        add("dve", 0.2, b3)
        add("act", 1.25, lambda: p.op("act", lambda h: h.activation(xr, accT, AF.Identity, bias=nm2, scale=rs2), rd=[racc] + rln2, wr=[rxr]))
        add("act", 1.15, lambda: p.op("act", lambda h: h.activation(xh, accT, AF.Identity, bias=nm2, scale=rs2), rd=[racc] + rln2, wr=[rxh]))
        add("dve", 1.2, lambda: p.op("dve", lambda h: h.tensor_tensor(xl, xr, xh, ALU.subtract), rd=[rxr, rxh], wr=[rxl]))
        u2f = xr.rearrange("p (a b) -> p a b", b=128)
        ul = xh.rearrange("p (a b) -> p a b", b=128)

        def f_tr():
            trs = trc[0] % 2
            trc[0] += 1

            def tr_slice(c):
                bk = TRS[trs][c // 4]
                return ps[:, 512 * bk + 128 * (c % 4):512 * bk + 128 * (c % 4) + 128]
            for c in range(8):
                osl = tr_slice(c)
                p.op("pe", lambda h, c=c, osl=osl: h.matmul(osl, xh[:, 128 * c:128 * c + 128], identb, start=True, stop=False),
                     rd=[rxh, r_identb], wr=[r_tr[trs]])
                p.op("pe", lambda h, c=c, osl=osl: h.matmul(osl, xl[:, 128 * c:128 * c + 128], identb, start=False, stop=True),
                     rd=[rxl, r_identb], wr=[r_tr[trs]])
            for c in (0, 1, 2, 4, 5, 6):
                src = tr_slice(c)
                p.op("act", lambda h, c=c, src=src: h.activation(u2f[:, c, :], src, AF.Identity,
                                                                 bias=modT[:, 2, c:c + 1], scale=modT[:, 3, c:c + 1]),
                     rd=[r_tr[trs], r_modT2], wr=[rxr])
            for c in (3, 7):
                src = tr_slice(c)
                p.op("dve", lambda h, c=c, src=src: h.tensor_scalar(u2f[:, c, :], src, modT[:, 3, c:c + 1], modT[:, 2, c:c + 1],
                                                                    ALU.mult, ALU.add),
                     rd=[r_tr[trs], r_modT2], wr=[rxr])
        add("pe", 1.9, f_tr, [("act", 3.4), ("dve", 0.9)])
        add("act", 1.15, lambda: p.op("act", lambda h: h.activation(u2T[:, :, 128 * T:128 * T + 128], u2f, AF.Copy), rd=[rxr], wr=[r_u2T[T]]))
        add("dve", 1.25, lambda: p.op("dve", lambda h: h.tensor_tensor(ul, u2f, u2T[:, :, 128 * T:128 * T + 128], ALU.subtract),
                                     rd=[rxr, r_u2T[T]], wr=[rxh]))

        def f_rt():
            k3 = 0
            for c in range(8):
                for (lh, rl) in ((0, wrh), (0, wrl), (1, wrh)):
                    lhs = (u2T[:, c, 128 * T:128 * T + 128] if lh == 0 else ul[:, c, :])
                    p.op("pe", lambda h, lhs=lhs, rl=rl, c=c, k3=k3: h.matmul(bank(LGB, 20 * T, 20 * T + 20), lhs, rl[:, c, :],
                                                                            start=(k3 == 0), stop=(k3 == 23)),
                         rd=[r_u2T[T], rxh, r_wrs], wr=[r_lg])
                    k3 += 1
        add("pe", 1.0, f_rt)
        _stop = int(os.environ.get("K_P3STOP", "1000"))
        return ops[:_stop]

    def list_schedule(chains, max_inflight, lat=0.6):
        n = len(chains)
        eng_free = {}
        ptr = [0] * n
        ready = [0.0] * n
        done = [0.0] * n
        active = []
        nxt = 0
        while True:
            while nxt < n and len(active) < max_inflight:
                ready[nxt] = done[nxt - max_inflight] if nxt >= max_inflight else 0.0
                active.append(nxt)
                nxt += 1
            if not active:
                break
            best = None
            for t in active:
                eng, dur, fn, extra = chains[t][ptr[t]]
                stt_ = max(eng_free.get(eng, 0.0), ready[t])
                if best is None or stt_ < best[0]:
                    best = (stt_, t)
            stt_, t = best
            eng, dur, fn, extra = chains[t][ptr[t]]
            if os.environ.get("K_P3DBG"):
                print("P3 emit tile", t, "op", ptr[t], eng, "t=%.1f" % stt_)
            fn()
            fin = stt_ + dur
            eng_free[eng] = fin
            for (e2, d2) in extra:
                st2 = max(eng_free.get(e2, 0.0), fin + lat)
                fin = st2 + d2
                eng_free[e2] = fin
            ready[t] = fin + lat
            ptr[t] += 1
            if ptr[t] == len(chains[t]):
                done[t] = ready[t]
                active.remove(t)

    list_schedule([tile_chain(T) for T in range(NOT)], NSET)

    p.alias(r_rt, r_xr + r_xh + r_xl)
    def bcl(v, n):
        return bass.AP(v.tensor, v.offset, [list(x) for x in v.ap] + [[0, n]])

    def bcm(v, n):
        a = [list(x) for x in v.ap]
        return bass.AP(v.tensor, v.offset, [a[0], [0, n]] + a[1:])

    def dv(fn, rd=(), wr_=None):
        p.op("dve", fn, rd=[r_rt] + list(rd), wr=[r_rt] if wr_ is None else wr_)

    lg3 = bank(LGB, 0, 20 * NOT).rearrange("p (a b) -> p a b", b=20)
    L4 = Lr[:, :, 0:4]
    dv(lambda h: h.tensor_tensor(Lr, lg3, bcm(brb, NOT), ALU.add), rd=[r_lg, r_brb])
    dv(lambda h: h.tensor_reduce(gmaxr, L4, AX.X, ALU.max))
    dv(lambda h: h.tensor_tensor(egr, L4, bcl(gmaxr, 4), ALU.subtract))
    p.op("act", lambda h: h.activation(egr, egr, AF.Exp), rd=[r_rt], wr=[r_rt])
    dv(lambda h: h.tensor_reduce(gsumr, egr, AX.X, ALU.add))
    dv(lambda h: h.reciprocal(gpr, gsumr))
    dv(lambda h: h.tensor_tensor(ogr, L4, bcl(gmaxr, 4), ALU.is_equal))
    dv(lambda h: h.tensor_scalar(penr, ogr, -1.0, BIG, ALU.add, ALU.mult))
    elm4 = elmr.rearrange("p a (g e) -> p a g e", e=4)
    L16 = Lr[:, :, 4:20].rearrange("p a (g e) -> p a g e", e=4)
    pen4 = bass.AP(penr.tensor, penr.offset, [list(x) for x in penr.ap] + [[0, 4]])
    dv(lambda h: h.tensor_tensor(elm4, L16, pen4, ALU.add))
    dv(lambda h: h.tensor_reduce(m1r, elmr, AX.X, ALU.max))
    dv(lambda h: h.tensor_tensor(o1r, elmr, bcl(m1r, 16), ALU.is_equal))
    dv(lambda h: h.scalar_tensor_tensor(elm2r, o1r, -BIG, elmr, ALU.mult, ALU.add))
    dv(lambda h: h.tensor_reduce(m2r, elm2r, AX.X, ALU.max))
    dv(lambda h: h.tensor_tensor(o2r, elm2r, bcl(m2r, 16), ALU.is_equal))
    dv(lambda h: h.tensor_tensor(ddr, m2r, m1r, ALU.subtract))
    p.op("act", lambda h: h.activation(edr, ddr, AF.Exp), rd=[r_rt], wr=[r_rt])
    dv(lambda h: h.tensor_scalar(w1r, edr, 1.0, None, ALU.add))
    dv(lambda h: h.reciprocal(w1r, w1r))
    dv(lambda h: h.scalar_tensor_tensor(w1r, w1r, 1.0 / ALPHA, gpr, ALU.mult, ALU.mult))
    dv(lambda h: h.tensor_tensor(w2r, edr, w1r, ALU.mult))
    dv(lambda h: h.tensor_tensor(o1r, o1r, bcl(w1r, 16), ALU.mult))
    dv(lambda h: h.tensor_tensor(o2r, o2r, bcl(w2r, 16), ALU.mult))
    dv(lambda h: h.tensor_tensor(combAll, o1r, o2r, ALU.add), wr_=[r_rt, r_comb])

    if phases < 4:
        return _finish_debug(nc, p, st, out_d, locals())

    r_wg = [R("wg0"), R("wg1")]
    r_wu = [R("wu0"), R("wu1")]
    r_wd = [R("wd0"), R("wd1")]
    olds = [x for row in r_QT for x in row] + [x for row in r_OP for x in row] + [x for row in r_OS for x in row]
    for r in r_wg + r_wu + r_wd:
        p.alias(r, olds + ph3_qt)
    o = W0
    sg, hT = [], []
    for k in range(2):
        t, o = carve(o, F32, [128, 512]); sg.append(t)
    for k in range(2):
        t, o = carve(o, BF16, [128, 4, 512]); hT.append(t)
    assert o <= WEND
    r_sg = [R("sg0"), R("sg1")]
    r_hT = [[R("hT%d_%d" % (k, hc)) for hc in range(4)] for k in range(2)]
    for r in r_sg + [x for row in r_hT for x in row]:
        p.alias(r, ph3_work + ph2_work + ph1_work)
    r_gb = [R("gb0"), R("gb1")]
    r_ub = [R("ub0"), R("ub1")]
    r_yb = [R("yb%d" % k) for k in range(4)]
    for r in r_gb + r_ub + r_yb:
        p.alias(r, ph3_ps)
    GB = [0, 1]
    UB = [2, 3]
    YB = [4, 5, 6, 7]

    r_bcA = R("bcA5")
    r_bcB = R("bcB5")
    for r in (r_bcA, r_bcB):
        p.alias(r, ph3_work + ph2_work + ph1_work)
    p.op("sp", lambda h: h.dma_start(out=bcA, in_=brow_d[4]), wr=[r_bcA], dma=r_bcA)
    p.op("sp", lambda h: h.dma_start(out=bcB, in_=brow_d[5]), wr=[r_bcB], dma=r_bcB)
    o = W0 + 12 * KB
    otb = []
    for k in range(2):
        t, o = carve(o, F32, [128, D]); otb.append(t)
    assert o <= W0 + 20 * KB
    r_ot = [R("ot0"), R("ot1")]
    for r in r_ot:
        p.alias(r, ph3_work + ph2_work + ph1_work)

    def final_tile(T):
        ot = otb[T % 2]
        rot = r_ot[T % 2]
        rs, nm, rln = ln_stats(acc[:, T, :], r_acc[T], EPS, eps_col=1)
        p.op("act", lambda h: h.activation(ot, acc[:, T, :], AF.Identity, bias=nm, scale=rs), rd=[r_acc[T]] + rln, wr=[rot])
        p.op("dve", lambda h: h.tensor_tensor(ot, ot, bcA, ALU.mult), rd=[r_bcA], wr=[rot])
        p.op("pool", lambda h: h.tensor_tensor(ot, ot, bcB, ALU.add), rd=[r_bcB], wr=[rot])
        p.op("sp", lambda h: h.dma_start(out=out_d[128 * T:128 * T + 128, :], in_=ot), rd=[rot], dma=rot)

    def load_expert(e):
        b = e % 2
        p.op("pool", lambda h: h.dma_start(out=wgb[b], in_=wg_d[e].rearrange("(c p) n -> p c n", p=128)),
             wr=[r_wg[b]], dma=r_wg[b])
        p.op("pool", lambda h: h.dma_start(out=wub[b], in_=wu_d[e].rearrange("(c p) n -> p c n", p=128)),
             wr=[r_wu[b]], dma=r_wu[b])
        p.op("pool", lambda h: h.dma_start(out=wdb[b], in_=wd_d[e].rearrange("(c p) n -> p c n", p=128)),
             wr=[r_wd[b]], dma=r_wd[b])
        for hc in range(4):
            p.op("pool", lambda h, hc=hc: h.tensor_tensor(wdb[b][:, hc, :], wdb[b][:, hc, :], g2b, ALU.mult),
                 rd=[r_g2b], wr=[r_wd[b]])

    load_expert(0)
    kk = 0
    yk = 0
    for e in range(NEXP):
        b = e % 2
        if e + 1 < NEXP:
            load_expert(e + 1)
        for tg in range(NTG):
            hb = (e * NTG + tg) % 2
            for hc in range(4):
                k2 = kk % 2
                kk += 1
                for c in range(8):
                    p.op("pe", lambda h, c=c, hc=hc, k2=k2, b=b, tg=tg: h.matmul(bank(GB[k2]), wgb[b][:, c, 128 * hc:128 * hc + 128],
                                                                   u2T[:, c, 512 * tg:512 * tg + 512], start=(c == 0), stop=(c == 7)),
                         rd=[r_wg[b]] + r_u2T[4 * tg:4 * tg + 4], wr=[r_gb[k2]])
                for c in range(8):
                    p.op("pe", lambda h, c=c, hc=hc, k2=k2, b=b, tg=tg: h.matmul(bank(UB[k2]), wub[b][:, c, 128 * hc:128 * hc + 128],
                                                                   u2T[:, c, 512 * tg:512 * tg + 512], start=(c == 0), stop=(c == 7)),
                         rd=[r_wu[b]] + r_u2T[4 * tg:4 * tg + 4], wr=[r_ub[k2]])
                p.op("act", lambda h, k2=k2: h.activation(sg[k2], bank(GB[k2]), AF.Silu), rd=[r_gb[k2]], wr=[r_sg[k2]])
                p.op("dve", lambda h, k2=k2, hc=hc, hb=hb: h.tensor_tensor(hT[hb][:, hc, :], sg[k2], bank(UB[k2]), ALU.mult),
                     rd=[r_sg[k2], r_ub[k2]], wr=[r_hT[hb][hc]])
            for tt in range(4):
                T = 4 * tg + tt
                for n2 in range(2):
                    y2 = yk % 4
                    yk += 1
                    for hc in range(4):
                        p.op("pe", lambda h, hc=hc, tt=tt, n2=n2, y2=y2, hb=hb, b=b: h.matmul(
                            bank(YB[y2]), hT[hb][:, hc, 128 * tt:128 * tt + 128], wdb[b][:, hc, 512 * n2:512 * n2 + 512],
                            start=(hc == 0), stop=(hc == 3)),
                            rd=[r_hT[hb][hc], r_wd[b]], wr=[r_yb[y2]])
                    asl = acc[:, T, 512 * n2:512 * n2 + 512]
                    p.op("dve", lambda h, asl=asl, y2=y2, T=T, e=e: h.scalar_tensor_tensor(
                        asl, bank(YB[y2]), combAll[:, T, e:e + 1], asl, ALU.mult, ALU.add),
                        rd=[r_yb[y2], r_comb], wr=[r_acc[T]])
                if e == NEXP - 1:
                    final_tile(T)

    p.op("sp", None, wr=r_ot)
    p.emit(nc)
    st.close()
    return nc


def _finish_debug(nc, p, st, out_d, L):
    dbg = L.get("_dbg")
    p.emit(nc)
    st.close()
    return nc


def _host_inputs(NSLOT, xb, cb, j, w):
    NV = 512 * NSLOT
    NT = 4 * NSLOT
    S = xb.shape[0]
    assert S == NV
    if j == 1:
        xvirt = np.ascontiguousarray(xb)
    else:
        xvirt = np.concatenate([np.zeros((256, D), np.float32), xb[:NV - 256]], axis=0)
    kb = np.zeros((128, NT), np.float32)
    if j == 0:
        kb[:, 0:2] = NEG
    valid = np.full((128, 1), 1.0 if j == 1 else 0.0, np.float32)
    kpad = np.full((128, 512), NEG if j == 0 else 0.0, np.float32)
    invc = np.zeros((128, 4, 256), np.float32)
    pos = np.arange(256) + 256 * j
    for g in range(4):
        wv = 2 << g
        invc[:, g, :] = (1.0 / np.minimum(pos + 1, wv)).astype(np.float32)[None, :]
    m = dict(w)
    m.update(xv=xvirt, cT=np.ascontiguousarray(cb.reshape(8, 128).T), kbias=kb, kpad=kpad, valid=valid, invc=invc)
    return m


def _shared_inputs(inp):
    f = np.float32
    b_ada = inp["b_ada"][0]
    bc = lambda v: np.ascontiguousarray(np.broadcast_to(np.asarray(v, f)[None, :], (128, D)))
    brow = np.stack([bc(b_ada[2 * D:3 * D]), bc(b_ada[5 * D:6 * D]), bc(inp["ln1_g"][0]), bc(inp["ln1_b"][0]),
                     bc(inp["ln2_g"][0]), bc(inp["ln2_b"][0])], axis=0)
    k = np.arange(128)[:, None]
    q = np.arange(128)[None, :]
    tri = np.where(k >= q, -8.0, 0.0).astype(f)
    ql = np.arange(256)[None, :]
    m0 = np.where(k < ql, 0.0, NEG).astype(f)
    m1 = np.where(k + 128 < ql, 0.0, NEG).astype(f)
    maskb = np.stack([np.concatenate([m0, m0], 1), np.concatenate([m1, m1], 1)], 0)
    w = dict(
        w_ada=np.ascontiguousarray(inp["w_ada"][0]), badaT=np.ascontiguousarray(b_ada.reshape(48, 128).T),
        brow=brow, w_in=np.ascontiguousarray(inp["w_in"][0]), w_pool=np.ascontiguousarray(inp["w_pool"][0]),
        pscT=np.ascontiguousarray(inp["pool_scale"][0].reshape(4, 128).T), w_out=np.ascontiguousarray(inp["w_out"][0]),
        wr=np.ascontiguousarray(np.concatenate([inp["w_router_group"][0], inp["w_router_expert"][0]], axis=1)),
        brb=np.ascontiguousarray(np.broadcast_to(np.concatenate([inp["b_router_group"][0], inp["b_router_expert"][0]])[None, :], (128, 20))),
        w_gate=np.ascontiguousarray(inp["w_gate"][0]), w_up=np.ascontiguousarray(inp["w_up"][0]),
        w_down=np.ascontiguousarray(inp["w_down"][0]),
        identf=np.eye(128, dtype=f), tri=tri, ones=np.ones((128, 128), f), onesn=np.full((128, 128), -8.0, f), maskb=maskb,
    )
    return {k_: np.asarray(v, f) for k_, v in w.items()}


_NC_CACHE = {}


def run_cores(inp, NSLOT, phases=5):
    x = np.asarray(inp["x"], np.float32)
    c = np.asarray(inp["c"], np.float32)
    B, S, _ = x.shape
    w = _shared_inputs(inp)
    in_maps = []
    for b in range(B):
        for j in range(2):
            in_maps.append(_host_inputs(NSLOT, x[b], c[b], j, w))
    key = (NSLOT, phases)
    if key not in _NC_CACHE:
        _NC_CACHE[key] = build(NSLOT, phases)
    nc = _NC_CACHE[key]
    res = run_bass_kernel_spmd(nc, in_maps, core_ids=list(range(2 * B)))
    out = np.zeros((B, S, D), np.float32)
    for b in range(B):
        for j in range(2):
            o = np.asarray(res.results[2 * b + j]["out"])
            for i in range(NSLOT):
                out[b, 256 * (2 * i + j):256 * (2 * i + j) + 256] = o[256 * i:256 * i + 256]
    return out


def kernel(**inputs):
    return run_cores(inputs, 8)
```
